# Optimizing a Trainium2 kernel written in Bass

```python
import jax, jax.numpy as jnp
from jax import lax
import numpy as np

D_MODEL = 2048
BATCH = 4
SEQ = 2048
DEPTH = 4

GRID_W = 64
CTX_LEN = 256
EPS = 1e-6

A_HEADS = 8
A_KDIM = 128
A_VDIM = 128
A_WIDTH = A_HEADS * A_KDIM
A_CHUNK = 64

POOL_WINDOWS = (2, 4, 8, 16)
B_WIDTH = 1024
B_GROUP = B_WIDTH // 4

C_HEADS = 8
C_HEAD_DIM = 128
C_WIDTH = C_HEADS * C_HEAD_DIM
NA_ROWS_MAX = 8
NA_COLS = 16
ROPE_THETA = 10000.0

N_BRANCH = 3
BRANCH_WIDTH = 1024
IN_SPLITS = (A_WIDTH, A_WIDTH, A_WIDTH, A_WIDTH, A_WIDTH, B_WIDTH, C_WIDTH, C_WIDTH, C_WIDTH, N_BRANCH * D_MODEL)
N_IN = 5 * A_WIDTH + B_WIDTH + 3 * C_WIDTH + N_BRANCH * D_MODEL

N_EXPERTS = 16
EXPERT_FF = 1024
EC_CAPACITY = 2

kernel_name = "hybrid_hgrn2_pool_natten_ec_dit"


def rmsnorm(t, gain):
    tf = t.astype(jnp.float32)
    y = tf * lax.rsqrt(jnp.mean(tf * tf, axis=-1, keepdims=True) + EPS) * gain.astype(jnp.float32)
    return y.astype(t.dtype)


def modulate(h, shift, scale):
    return h * (1 + scale) + shift


def split_columns(u):
    offs = np.cumsum(np.array(IN_SPLITS))[:-1].tolist()
    return jnp.split(u, offs, axis=-1)


def to_heads(t, n_heads):
    b, l, w = t.shape
    return t.reshape(b, l, n_heads, w // n_heads).transpose(0, 2, 1, 3)


def flip_seq(t):
    return jnp.flip(t, axis=2)


def hgrn_gates(z, lb):
    z = z.astype(jnp.float32)
    lb = lb.astype(jnp.float32)
    logf = jnp.logaddexp(jnp.log(lb), jnp.log1p(-lb) + jax.nn.log_sigmoid(z))
    k = (1 - lb) * jax.nn.sigmoid(-z)
    return to_heads(logf, A_HEADS), to_heads(k, A_HEADS)


def hgrn_chunk_scan(q, logf, k, v, s0):
    bsz, h, n, dk = q.shape
    dv = v.shape[-1]
    nc = n // A_CHUNK

    def to_chunks(t):
        return jnp.moveaxis(t.reshape(bsz, h, nc, A_CHUNK, t.shape[-1]), 2, 0)

    lower = jnp.tril(jnp.ones((A_CHUNK, A_CHUNK), dtype=bool))

    def step(s, blk):
        qc, gc, kc, vc = blk
        b = jnp.cumsum(gc, axis=2)
        diff = jnp.where(lower[:, :, None], b[:, :, :, None, :] - b[:, :, None, :, :], -jnp.inf)
        scores = jnp.einsum('bhtk,bhsk,bhtsk->bhts', qc, kc, jnp.exp(diff))
        o = jnp.einsum('bhtk,bhkv->bhtv', qc * jnp.exp(b), s) + jnp.einsum('bhts,bhsv->bhtv', scores, vc)
        b_last = b[:, :, -1:, :]
        s_new = jnp.exp(b_last[:, :, 0, :, None]) * s + jnp.einsum('bhsk,bhsv->bhkv', kc * jnp.exp(b_last - b), vc)
        return s_new, o

    s_fin, o = lax.scan(step, s0, (to_chunks(q), to_chunks(logf), to_chunks(k), to_chunks(v)))
    o = jnp.moveaxis(o, 0, 2).reshape(bsz, h, n, dv)
    return o, s_fin


def hgrn_final_state(logf, k, v):
    b = jnp.cumsum(logf, axis=2)
    return jnp.einsum('bhsk,bhsv->bhkv', k * jnp.exp(b[:, :, -1:] - b), v)


def hgrn_bidir(u_q, u_ff, u_fb, u_i, lb_f, lb_b, s_f0, s_b0):
    q = to_heads(jax.nn.silu(u_q), A_HEADS)
    v = to_heads(u_i, A_HEADS)
    lf_f, k_f = hgrn_gates(u_ff, lb_f)
    lf_b, k_b = hgrn_gates(u_fb, lb_b)
    o_f, s_f = hgrn_chunk_scan(q, lf_f, k_f, v, s_f0)
    o_b, s_b = hgrn_chunk_scan(flip_seq(q), flip_seq(lf_b), flip_seq(k_b), flip_seq(v), s_b0)
    return o_f + flip_seq(o_b), s_f, s_b


def hgrn_context_states(u_ff, u_fb, u_i, lb_f, lb_b):
    v = to_heads(u_i, A_HEADS)
    lf_f, k_f = hgrn_gates(u_ff, lb_f)
    lf_b, k_b = hgrn_gates(u_fb, lb_b)
    s_f = hgrn_final_state(lf_f, k_f, v)
    s_b = hgrn_final_state(flip_seq(lf_b), flip_seq(k_b), flip_seq(v))
    return s_f, s_b


def hgrn_readout(o, g, gain):
    bsz, h, l, dv = o.shape
    o = o.astype(jnp.float32).transpose(0, 2, 1, 3)
    o = o * lax.rsqrt(jnp.mean(o * o, axis=-1, keepdims=True) + EPS) * gain.astype(jnp.float32).reshape(h, dv)
    return (o.reshape(bsz, l, h * dv) * jax.nn.silu(g.astype(jnp.float32))).astype(g.dtype)


def multiscale_pool(u, w_pool, scale):
    bsz, n, _ = u.shape
    uf = u.astype(jnp.float32)
    csum = jnp.concatenate([jnp.zeros((bsz, 1, B_WIDTH), jnp.float32), jnp.cumsum(uf, axis=1)], axis=1)
    pos = np.arange(n)
    diffs = []
    for gi, w in enumerate(POOL_WINDOWS):
        lo = np.clip(pos - w // 2, 0, n - 1)
        hi = np.clip(pos + w // 2 - 1, 0, n - 1)
        cnt = (hi - lo + 1).astype(np.float32)[None, :, None]
        seg = csum[:, :, gi * B_GROUP:(gi + 1) * B_GROUP]
        mean = (seg[:, hi + 1] - seg[:, lo]) / cnt
        diffs.append(mean - uf[:, :, gi * B_GROUP:(gi + 1) * B_GROUP])
    d = jnp.stack(diffs, axis=2)
    y = jnp.einsum('blgc,gce->blge', d, w_pool.astype(jnp.float32)).reshape(bsz, n, B_WIDTH)
    return (y * scale.astype(jnp.float32)).astype(u.dtype)


def axial_rope(t):
    n = t.shape[1]
    pos = np.arange(n)
    row = jnp.asarray(pos // GRID_W, jnp.float32)
    col = jnp.asarray(pos % GRID_W, jnp.float32)
    half = C_HEAD_DIM // 2
    inv_freq = ROPE_THETA ** (-jnp.arange(0, half, 2, dtype=jnp.float32) / half)

    def rot(u, p):
        ang = p[:, None] * inv_freq
        cos = jnp.cos(ang)[None, :, None, :]
        sin = jnp.sin(ang)[None, :, None, :]
        u1, u2 = u[..., :half // 2], u[..., half // 2:]
        return jnp.concatenate([u1 * cos - u2 * sin, u2 * cos + u1 * sin], axis=-1)

    tf = t.astype(jnp.float32)
    return jnp.concatenate([rot(tf[..., :half], row), rot(tf[..., half:], col)], axis=-1).astype(t.dtype)


def neighborhood_attention(q, k, v, k_ctx, v_ctx, rpb):
    bsz, n, h, dh = q.shape
    rows = n // GRID_W
    kr = min(NA_ROWS_MAX, rows)
    r = np.arange(rows)
    key_rows = np.clip(r - kr // 2, 0, rows - kr)[:, None] + np.arange(kr)[None, :]
    qcol = np.arange(GRID_W)
    col_start = np.clip(qcol - NA_COLS // 2, 0, GRID_W - NA_COLS)
    kcol = np.arange(GRID_W)
    col_mask = (kcol[None, :] >= col_start[:, None]) & (kcol[None, :] < col_start[:, None] + NA_COLS)
    mask = np.broadcast_to(col_mask[:, None, :], (GRID_W, kr, GRID_W)).reshape(GRID_W, kr * GRID_W)
    dr_idx = key_rows - r[:, None] + NA_ROWS_MAX - 1
    dc_idx = np.clip(kcol[None, :] - qcol[:, None] + NA_COLS - 1, 0, 2 * NA_COLS - 2)
    bias = rpb.astype(jnp.float32)[:, dr_idx][..., dc_idx]
    bias = bias.transpose(0, 1, 3, 2, 4).reshape(h, rows, GRID_W, kr * GRID_W)

    scale = dh ** -0.5
    qg = q.reshape(bsz, rows, GRID_W, h, dh)
    kg = k.reshape(bsz, rows, GRID_W, h, dh)[:, key_rows].reshape(bsz, rows, kr * GRID_W, h, dh)
    vg = v.reshape(bsz, rows, GRID_W, h, dh)[:, key_rows].reshape(bsz, rows, kr * GRID_W, h, dh)
    s_loc = jnp.einsum('brqhd,brnhd->bhrqn', qg, kg).astype(jnp.float32) * scale + bias[None]
    s_loc = jnp.where(mask, s_loc, -jnp.inf)
    s_ctx = jnp.einsum('brqhd,bchd->bhrqc', qg, k_ctx).astype(jnp.float32) * scale
    p = jax.nn.softmax(jnp.concatenate([s_loc, s_ctx], axis=-1), axis=-1).astype(v.dtype)
    n_loc = kr * GRID_W
    out = jnp.einsum('bhrqn,brnhd->brqhd', p[..., :n_loc], vg) + jnp.einsum('bhrqc,bchd->brqhd', p[..., n_loc:], v_ctx)
    return out.reshape(bsz, n, h * dh)


def context_attention(q, k, v):
    bsz, lc, h, dh = q.shape
    s = jnp.einsum('bqhd,bkhd->bhqk', q, k).astype(jnp.float32) * (dh ** -0.5)
    p = jax.nn.softmax(s, axis=-1).astype(v.dtype)
    return jnp.einsum('bhqk,bkhd->bqhd', p, v).reshape(bsz, lc, h * dh)


def merge_branches(ys, u_gate, w_branch, w_out):
    bsz, l, _ = u_gate.shape
    y = jnp.stack(ys, axis=2)
    proj = jnp.einsum('bljw,jwd->bljd', y, w_branch)
    gates = jax.nn.sigmoid(u_gate.reshape(bsz, l, N_BRANCH, D_MODEL))
    return jnp.einsum('bld,de->ble', jnp.sum(gates * proj, axis=2), w_out)


def expert_choice_ffn(h, w_router, w_gate, w_up, w_down):
    bsz, n, d = h.shape
    cap = EC_CAPACITY * n // N_EXPERTS
    aff = jax.nn.softmax((h @ w_router).astype(jnp.float32), axis=-1)
    g, idx = lax.top_k(jnp.swapaxes(aff, 1, 2), cap)
    xg = jax.vmap(lambda hb, ib: hb[ib])(h, idx)
    hid = jax.nn.silu(jnp.einsum('becd,edf->becf', xg, w_gate)) * jnp.einsum('becd,edf->becf', xg, w_up)
    y = jnp.einsum('becf,efd->becd', hid, w_down) * g[..., None].astype(h.dtype)
    return jax.vmap(lambda yb, ib: jnp.zeros((n, d), h.dtype).at[ib.reshape(-1)].add(yb.reshape(-1, d)))(y, idx)


def setup_inputs(seed: int = 0) -> dict:
    key = jax.random.key(seed)
    ks = jax.random.split(key, 21)
    f32 = jnp.float32

    def nrm(k, shape, s):
        return jax.random.normal(k, shape, f32) * s

    L, D = DEPTH, D_MODEL
    return {
        "x": nrm(ks[0], (BATCH, SEQ, D), 1.0),
        "c": nrm(ks[1], (BATCH, D), 1.0),
        "ctx": nrm(ks[2], (BATCH, CTX_LEN, D), 1.0),
        "c_ctx": nrm(ks[3], (D,), 1.0),
        "w_mod": nrm(ks[4], (L, D, 6 * D), 0.5 * D ** -0.5),
        "b_mod": nrm(ks[5], (L, 6 * D), 0.02),
        "g_norm1": 1.0 + nrm(ks[6], (L, D), 0.05),
        "w_in": nrm(ks[7], (L, D, N_IN), D ** -0.5),
        "lb_param": nrm(ks[8], (2, L, A_WIDTH), 1.0),
        "g_hgrn": 1.0 + nrm(ks[9], (L, A_WIDTH), 0.05),
        "w_pool": nrm(ks[10], (L, 4, B_GROUP, B_GROUP), B_GROUP ** -0.5),
        "pool_scale": 1.0 + nrm(ks[11], (L, B_WIDTH), 0.05),
        "rpb": nrm(ks[12], (L, C_HEADS, 2 * NA_ROWS_MAX - 1, 2 * NA_COLS - 1), 0.1),
        "w_branch": nrm(ks[13], (L, N_BRANCH, BRANCH_WIDTH, D), BRANCH_WIDTH ** -0.5),
        "w_out": nrm(ks[14], (L, D, D), D ** -0.5),
        "g_norm2": 1.0 + nrm(ks[15], (L, D), 0.05),
        "w_router": nrm(ks[16], (L, D, N_EXPERTS), D ** -0.5),
        "w_gate_e": nrm(ks[17], (L, N_EXPERTS, D, EXPERT_FF), D ** -0.5),
        "w_up_e": nrm(ks[18], (L, N_EXPERTS, D, EXPERT_FF), D ** -0.5),
        "w_down_e": nrm(ks[19], (L, N_EXPERTS, EXPERT_FF, D), EXPERT_FF ** -0.5),
        "g_final": 1.0 + nrm(ks[20], (D,), 0.05),
    }


def reference(x, c, ctx, c_ctx, w_mod, b_mod, g_norm1, w_in, lb_param, g_hgrn, w_pool, pool_scale, rpb,
              w_branch, w_out, g_norm2, w_router, w_gate_e, w_up_e, w_down_e, g_final):
    bsz, n, _ = x.shape
    lc = ctx.shape[1]
    lb_all = jnp.cumsum(jax.nn.softmax(lb_param.astype(jnp.float32), axis=1), axis=1)
    lb_all = lb_all - lb_all[:, :1]
    sc = jax.nn.silu(c)
    scc = jax.nn.silu(c_ctx)
    xc = ctx
    for l in range(DEPTH):
        last = l == DEPTH - 1
        mod_x = jnp.split((sc @ w_mod[l] + b_mod[l])[:, None, :], 6, axis=-1)
        mod_c = jnp.split(scc @ w_mod[l] + b_mod[l], 6, axis=-1)
        lb_f = lb_all[0, l]
        lb_b = lb_all[1, l]

        hc = modulate(rmsnorm(xc, g_norm1[l]), mod_c[0], mod_c[1])
        cq_a, cf_f, cf_b, ci_a, cg_a, cu_b, cq_c, ck_c, cv_c, cu_gate = split_columns(hc @ w_in[l])
        ck_h = ck_c.reshape(bsz, lc, C_HEADS, C_HEAD_DIM)
        cv_h = cv_c.reshape(bsz, lc, C_HEADS, C_HEAD_DIM)
        if last:
            s_f, s_b = hgrn_context_states(cf_f, cf_b, ci_a, lb_f, lb_b)
        else:
            zero = jnp.zeros((bsz, A_HEADS, A_KDIM, A_VDIM), jnp.float32)
            co_a, s_f, s_b = hgrn_bidir(cq_a, cf_f, cf_b, ci_a, lb_f, lb_b, zero, zero)
            cy_a = hgrn_readout(co_a, cg_a, g_hgrn[l])
            cy_b = multiscale_pool(cu_b, w_pool[l], pool_scale[l])
            cy_c = context_attention(cq_c.reshape(bsz, lc, C_HEADS, C_HEAD_DIM), ck_h, cv_h)
            xc_mid = xc + mod_c[2] * merge_branches((cy_a, cy_b, cy_c), cu_gate, w_branch[l], w_out[l])

        h = modulate(rmsnorm(x, g_norm1[l]), mod_x[0], mod_x[1])
        q_a, f_f, f_b, i_a, g_a, u_b, q_c, k_c, v_c, u_gate = split_columns(h @ w_in[l])
        o_a, _, _ = hgrn_bidir(q_a, f_f, f_b, i_a, lb_f, lb_b, s_f, s_b)
        y_a = hgrn_readout(o_a, g_a, g_hgrn[l])
        y_b = multiscale_pool(u_b, w_pool[l], pool_scale[l])
        qh = axial_rope(q_c.reshape(bsz, n, C_HEADS, C_HEAD_DIM))
        kh = axial_rope(k_c.reshape(bsz, n, C_HEADS, C_HEAD_DIM))
        vh = v_c.reshape(bsz, n, C_HEADS, C_HEAD_DIM)
        y_c = neighborhood_attention(qh, kh, vh, ck_h, cv_h, rpb[l])
        x = x + mod_x[2] * merge_branches((y_a, y_b, y_c), u_gate, w_branch[l], w_out[l])
        h2 = modulate(rmsnorm(x, g_norm2[l]), mod_x[3], mod_x[4])
        x = x + mod_x[5] * expert_choice_ffn(h2, w_router[l], w_gate_e[l], w_up_e[l], w_down_e[l])

        if not last:
            hc2 = modulate(rmsnorm(xc_mid, g_norm2[l]), mod_c[3], mod_c[4])
            xc = xc_mid + mod_c[5] * expert_choice_ffn(hc2, w_router[l], w_gate_e[l], w_up_e[l], w_down_e[l])
    return rmsnorm(x, g_final)
```

```python
import numpy as np
from contextlib import ExitStack
import concourse.bass as bass
import concourse.mybir as mybir
from concourse.bass_utils import run_bass_kernel_spmd

F32 = mybir.dt.float32
BF16 = mybir.dt.bfloat16
I32 = mybir.dt.int32
AF = mybir.ActivationFunctionType
ALU = mybir.AluOpType

D = 2048
NL = 2048
NCX = 256
T = NL + NCX
DC = 16
L = 4
NIN = 15360
NE = 16
FF = 1024
CAPL = 256
CAPC = 32
NS = CAPL + CAPC
EPS = 1e-6
CH = 64
NCHUNK = T // CH
TBLK = [(0, 512), (512, 512), (1024, 512), (1536, 512), (2048, 256)]
ENGS = ("pe", "dve", "act", "pool", "sp")
NEG = -30000.0


class Buf:
    __slots__ = ("name", "w", "r")

    def __init__(self, name):
        self.name = name
        self.w = None
        self.r = {}


class Prog:
    def __init__(self, nc):
        self.nc = nc
        self.E = {"pe": nc.tensor, "dve": nc.vector, "act": nc.scalar, "pool": nc.gpsimd, "sp": nc.sync}
        self.sem = {}
        self.cnt = {}
        self.waited = {e: {} for e in ENGS}
        self.bufs = {}
        self.nops = 0
        for e in ENGS:
            self._mk(e)

    def _mk(self, k):
        if k not in self.sem:
            self.sem[k] = self.nc.alloc_semaphore(name="s_" + k)
            self.cnt[k] = 0

    def buf(self, x):
        if isinstance(x, Buf):
            return x
        n = x.tensor.name
        b = self.bufs.get(n)
        if b is None:
            b = self.bufs[n] = Buf(n)
        return b

    def _sync(self, eng, reads, writes, acc=False):
        need = {}
        for b in reads:
            if b.w is not None:
                k, v = b.w
                if need.get(k, 0) < v:
                    need[k] = v
        if not acc:
            for b in writes:
                if b.w is not None:
                    k, v = b.w
                    if need.get(k, 0) < v:
                        need[k] = v
                for k, v in b.r.items():
                    if need.get(k, 0) < v:
                        need[k] = v
        wd = self.waited[eng]
        e = self.E[eng]
        for k, v in need.items():
            if wd.get(k, 0) >= v:
                continue
            if k.startswith("d_"):
                v = self.cnt[k]
            wd[k] = v
            e.wait_ge(self.sem[k], v)

    def _commit(self, tok, reads, writes):
        k, v = tok
        for b in reads:
            if b.r.get(k, 0) < v:
                b.r[k] = v
        for b in writes:
            b.w = tok
            b.r = {}

    def op(self, eng, fn, r=(), w=(), acc=False):
        rb = [self.buf(x) for x in r]
        wb = [self.buf(x) for x in w]
        self._sync(eng, rb, wb, acc)
        ins = fn(self.E[eng])
        self.cnt[eng] += 1
        ins.then_inc(self.sem[eng], 1)
        self._commit((eng, self.cnt[eng]), rb, wb)
        self.nops += 1

    def dma(self, q, out, in_, key=None, r=None, w=None):
        rb = [self.buf(x) for x in (r if r is not None else [in_])]
        wb = [self.buf(x) for x in (w if w is not None else [out])]
        if key is None:
            key = q + ("_st" if "DRam" in type(out.tensor).__name__ else "_ld")
        k = "d_" + key
        self._mk(k)
        self._sync(q, rb, wb)
        ins = self.E[q].dma_start(out=out, in_=in_)
        self.cnt[k] += 16
        ins.then_inc(self.sem[k], 16)
        self._commit((k, self.cnt[k]), rb, wb)
        self.nops += 1

    def barrier(self):
        for e in ENGS:
            wd = self.waited[e]
            for k, v in self.cnt.items():
                if v > 0 and wd.get(k, 0) < v:
                    wd[k] = v
                    self.E[e].wait_ge(self.sem[k], v)

    def mm(self, out, lhsT, rhs, start, stop):
        self.op("pe", lambda e: e.matmul(out, lhsT, rhs, start=start, stop=stop),
                r=[lhsT, rhs], w=[out], acc=not start)

    def tr(self, out, in_, ident):
        self.op("pe", lambda e: e.transpose(out, in_, ident), r=[in_, ident], w=[out])

    def act(self, out, in_, func, scale=None, bias=None, eng="act"):
        r = [in_]
        kw = {}
        if scale is not None:
            kw["scale"] = scale
            if not isinstance(scale, (int, float)):
                r.append(scale)
        if bias is not None:
            kw["bias"] = bias
            if not isinstance(bias, (int, float)):
                r.append(bias)
        self.op("act", lambda e: e.activation(out=out, in_=in_, func=func, **kw), r=r, w=[out])

    def tt(self, eng, out, in0, in1, op):
        self.op(eng, lambda e: e.tensor_tensor(out=out, in0=in0, in1=in1, op=op), r=[in0, in1], w=[out])

    def ts(self, eng, out, in0, s1, s2, op0, op1=None):
        r = [in0]
        if not isinstance(s1, (int, float)):
            r.append(s1)
        if s2 is not None and not isinstance(s2, (int, float)):
            r.append(s2)
        if op1 is None:
            self.op(eng, lambda e: e.tensor_scalar(out=out, in0=in0, scalar1=s1, scalar2=None, op0=op0), r=r, w=[out])
        else:
            self.op(eng, lambda e: e.tensor_scalar(out=out, in0=in0, scalar1=s1, scalar2=s2, op0=op0, op1=op1), r=r, w=[out])

    def stt(self, out, in0, scalar, in1, op0, op1):
        r = [in0, in1]
        if not isinstance(scalar, (int, float)):
            r.append(scalar)
        self.op("dve", lambda e: e.scalar_tensor_tensor(out=out, in0=in0, scalar=scalar, in1=in1, op0=op0, op1=op1),
                r=r, w=[out])

    def copy(self, eng, out, in_):
        if eng == "act":
            self.op("act", lambda e: e.copy(out=out, in_=in_), r=[in_], w=[out])
        else:
            self.op(eng, lambda e: e.tensor_copy(out=out, in_=in_), r=[in_], w=[out])

    def memset(self, eng, out, val):
        self.op(eng, lambda e: e.memset(out, val), r=[], w=[out])


class Ring:
    def __init__(self, st, nc, name, n, shape, dtype, psum=False):
        self.t = []
        self.i = 0
        for k in range(n):
            alloc = nc.psum_tensor if psum else nc.sbuf_tensor
            self.t.append(st.enter_context(alloc(f"{name}{k}", list(shape), dtype)))

    def next(self):
        t = self.t[self.i % len(self.t)]
        self.i += 1
        return t


def _consts():
    c = {}
    c["ident"] = np.eye(128, dtype=np.float32)
    s = np.arange(CH)
    c["maskf"] = (s[:, None] <= s[None, :]).astype(np.int32)
    c["maskb"] = (s[:, None] >= s[None, :]).astype(np.int32)
    c["iotar"] = np.broadcast_to(np.arange(NS, dtype=np.float32)[None, :], (128, NS)).copy()
    p = np.arange(128, dtype=np.float32)
    c["iotac"] = np.stack([p, p + 128, p + 256], axis=1).copy()
    pos = np.arange(NL)
    row = (pos // 64).astype(np.float32)
    col = (pos % 64).astype(np.float32)
    half = 64
    inv_freq = (10000.0 ** (-np.arange(0, half, 2, dtype=np.float32) / half)).astype(np.float32)
    cosT = np.ones((128, T), np.float32)
    sinT = np.zeros((128, T), np.float32)
    ar = row[None, :] * inv_freq[:, None]
    ac = col[None, :] * inv_freq[:, None]
    cosT[0:32, :NL] = np.cos(ar); cosT[32:64, :NL] = np.cos(ar)
    cosT[64:96, :NL] = np.cos(ac); cosT[96:128, :NL] = np.cos(ac)
    sinT[0:32, :NL] = np.sin(ar); sinT[32:64, :NL] = np.sin(ar)
    sinT[64:96, :NL] = np.sin(ac); sinT[96:128, :NL] = np.sin(ac)
    c["cosT"] = cosT
    c["sinT"] = sinT
    R = np.zeros((128, 128), np.float32)
    for i in range(32):
        R[i, 32 + i] = -1.0
        R[32 + i, i] = 1.0
        R[64 + i, 96 + i] = -1.0
        R[96 + i, 64 + i] = 1.0
    c["RT"] = R.T.copy()
    inv = np.zeros((4, T), np.float32)
    for gi, w in enumerate((2, 4, 8, 16)):
        for (o, n) in ((0, NL), (NL, NCX)):
            ps = np.arange(n)
            lo = np.clip(ps - w // 2, 0, n - 1)
            hi = np.clip(ps + w // 2 - 1, 0, n - 1)
            inv[gi, o:o + n] = 1.0 / (hi - lo + 1)
    c["invcnt"] = np.broadcast_to(inv[:, None, :], (4, 128, T)).copy()
    c["attmask"], _, _ = _att_tables()
    sel = np.zeros((16, 16, 128), np.float32)
    for e in range(16):
        sel[e, e, :] = 1.0
    c["sel"] = sel
    return c


def _att_tiles():
    tiles = []
    for j in range(6):
        tiles.append(("I", j))
    for j in range(4):
        tiles.append(("A", j))
    for j in range(4):
        tiles.append(("Z", j))
    return tiles


def _att_tables():
    tiles = _att_tiles()
    mask = np.zeros((128, 14, 256), np.float32)
    dri = np.zeros((14, 128, 256), np.int64)
    dci = np.zeros((14, 128, 256), np.int64)
    kcol = np.arange(64)
    qcol = np.arange(64)
    cs = np.clip(qcol - 8, 0, 48)
    inwin = (kcol[:, None] >= cs[None, :]) & (kcol[:, None] < cs[None, :] + 16)
    dc = np.clip(kcol[:, None] - qcol[None, :] + 15, 0, 30)
    for ti, (kind, j) in enumerate(tiles):
        for a in range(2):
            for b in range(4):
                if kind == "I":
                    krp = 2 * j + a
                    valid = (b <= krp) and (krp < b + 8)
                    dr = krp - b - 4
                elif kind == "A":
                    valid = True
                    dr = 2 * j + a - b
                else:
                    valid = True
                    dr = 2 * j + a - b - 4
                drc = int(np.clip(dr + 7, 0, 14))
                blk = np.where(inwin, 0.0, NEG) if valid else np.full((64, 64), NEG)
                mask[a * 64:(a + 1) * 64, ti, b * 64:(b + 1) * 64] = blk
                dri[ti, a * 64:(a + 1) * 64, b * 64:(b + 1) * 64] = drc
                dci[ti, a * 64:(a + 1) * 64, b * 64:(b + 1) * 64] = dc
    return mask, dri, dci


def build(stop_after=99, dbg=(), nlayers=L):
    nc = bass.Bass("TRN2", target_bir_lowering=False)
    st = ExitStack()
    P = Prog(nc)

    class _NCW:
        n = 0

        def sbuf_tensor(self, name, shape, dt):
            _NCW.n += 1
            return nc.sbuf_tensor(f"{name}_u{_NCW.n}", shape, dt)

        def psum_tensor(self, name, shape, dt):
            _NCW.n += 1
            return nc.psum_tensor(f"{name}_u{_NCW.n}", shape, dt)
    ncw = _NCW()
    outs = {}

    def din(name, shape, dt=F32):
        return nc.dram_tensor(name, list(shape), dt, kind="ExternalInput").ap()

    def dscr(name, shape, dt):
        kind = "ExternalOutput" if name in dbg else "Internal"
        return nc.dram_tensor(name, list(shape), dt, kind=kind).ap()

    x_in = din("x", [NL, D])
    ctx_in = din("ctx", [NCX, D])
    cc_in = din("cc", [32, 128])
    w_mod = din("w_mod", [nlayers, D, 6 * D])
    vecs_in = din("vecs", [L, 144, 128])
    lbp_in = din("lbp", [64, 128])
    gfin_in = din("gfin", [16, 128])
    w_in = din("w_in", [nlayers, D, NIN])
    w_pool = din("w_pool", [nlayers, 4, 256, 256])
    rpbg = din("rpbg", [nlayers, 8, 128, 14, 256])
    w_branch = din("w_branch", [nlayers, 3, 1024, D])
    w_out = din("w_out", [nlayers, D, D])
    w_router = din("w_router", [nlayers, D, NE])
    w_gate = din("w_gate_e", [nlayers, NE, D, FF])
    w_up = din("w_up_e", [nlayers, NE, D, FF])
    w_down = din("w_down_e", [nlayers, NE, FF, D])
    cst = _consts()
    cin = {k: din("k_" + k, v.shape, I32 if v.dtype == np.int32 else F32) for k, v in cst.items()}
    y_out = nc.dram_tensor("y", [NL, D], F32, kind="ExternalOutput").ap()

    XT = [dscr(f"XT{c}", [128, T], F32) for c in range(DC)]
    QA = [dscr(f"QA{h}", [128, T], BF16) for h in range(8)]
    KF = [dscr(f"KF{h}", [128, T], BF16) for h in range(8)]
    KB = [dscr(f"KB{h}", [128, T], BF16) for h in range(8)]
    LFF = [dscr(f"LFF{h}", [128, T], F32) for h in range(8)]
    LFB = [dscr(f"LFB{h}", [128, T], F32) for h in range(8)]
    GA = [dscr(f"GA{h}", [128, T], BF16) for h in range(8)]
    UB = [dscr(f"UB{h}", [128, T], F32) for h in range(8)]
    QC = [dscr(f"QC{h}", [128, T], BF16) for h in range(8)]
    KC = [dscr(f"KC{h}", [128, T], BF16) for h in range(8)]
    VT = dscr("VT", [T, 1024], BF16)
    VC = dscr("VC", [T, 1024], BF16)
    UG = [dscr(f"UG{j}", [128, T], BF16) for j in range(48)]
    YA = [dscr(f"YA{h}", [128, T], BF16) for h in range(8)]
    YB = [dscr(f"YB{h}", [128, T], BF16) for h in range(8)]
    YC = [dscr(f"YC{h}", [128, T], BF16) for h in range(8)]
    MG = [dscr(f"MG{c}", [128, T], BF16) for c in range(DC)]
    YM = [dscr(f"YM{c}", [128, NE * 3 * 128], BF16) for c in range(DC)]

    def sb(name, shape, dt=F32):
        return st.enter_context(ncw.sbuf_tensor(name, list(shape), dt))

    ident = sb("ident", [128, 128])
    identb = sb("identb", [128, 128], BF16)
    onesb = sb("onesb", [128, 128], BF16)
    modT = sb("modT", [128, L, 96, 2])
    vecT = sb("vecT", [128, L, 144])
    lbT = sb("lbT", [128, 2, L, 8])
    oml = sb("oml", [128, 2, L, 8])
    gfT = sb("gfT", [128, 16])
    A1 = sb("A1", [128, L, 16, 2]); A2 = sb("A2", [128, L, 16, 2])
    pall = Ring(st, ncw, "pp", 8, [128, 512], F32, psum=True)
    pps = Ring.__new__(Ring); pps.t = pall.t[0:4]; pps.i = 0
    pacc = Ring.__new__(Ring); pacc.t = pall.t[4:8]; pacc.i = 0

    P.dma("sp", ident[:], cin["ident"][:, :])
    P.copy("dve", identb[:], ident[:])
    P.memset("dve", onesb[:], 1.0)

    with ExitStack() as ph:
        def psb(name, shape, dt=F32):
            return ph.enter_context(ncw.sbuf_tensor(name, list(shape), dt))
        xin = Ring(ph, ncw, "xin", 2, [128, D], F32)
        xst = Ring(ph, ncw, "xst", 2, [128, DC, 128], F32)
        for tt_ in range(T // 128):
            xt = xin.next()
            src = x_in[tt_ * 128:(tt_ + 1) * 128, :] if tt_ < 16 else ctx_in[(tt_ - 16) * 128:(tt_ - 15) * 128, :]
            P.dma("sp", xt[:], src)
            stg = xst.next()
            for g in range(4):
                ps = pps.next()
                for j in range(4):
                    c = g * 4 + j
                    P.tr(ps[:, j * 128:(j + 1) * 128], xt[:, c * 128:(c + 1) * 128], ident[:])
                P.copy("dve" if g % 2 == 0 else "act", stg[:, g * 4:(g + 1) * 4, :],
                       ps[:].rearrange("p (a b) -> p a b", a=4))
            for c in range(DC):
                P.dma("sp", XT[c][:, tt_ * 128:(tt_ + 1) * 128], stg[:, c, :])
        vtmp = psb("vtmp", [128, 128])
        for l in range(L):
            for (r0, nr) in ((0, 96), (96, 48)):
                P.dma("sp", vtmp[0:nr, :], vecs_in[l, r0:r0 + nr, :])
                ps = pps.next()
                P.tr(ps[:, 0:nr], vtmp[0:nr, :], ident[0:nr, 0:nr])
                P.copy("dve", vecT[:, l, r0:r0 + nr], ps[:, 0:nr])
        P.dma("sp", vtmp[0:64, :], lbp_in[:, :])
        ps = pps.next()
        P.tr(ps[:, 0:64], vtmp[0:64, :], ident[0:64, 0:64])
        lbe = psb("lbe", [128, 2, L, 8])
        P.act(lbe[:].rearrange("p a b c -> p (a b c)"), ps[:, 0:64], AF.Exp)
        lsum = psb("lsum", [128, 2, 8])
        P.tt("dve", lsum[:], lbe[:, :, 0, :], lbe[:, :, 1, :], ALU.add)
        P.tt("dve", lsum[:], lsum[:], lbe[:, :, 2, :], ALU.add)
        P.tt("dve", lsum[:], lsum[:], lbe[:, :, 3, :], ALU.add)
        lrec = psb("lrec", [128, 2, 8])
        P.op("dve", lambda e: e.reciprocal(out=lrec[:], in_=lsum[:]), r=[lsum[:]], w=[lrec[:]])
        P.memset("dve", lbT[:, :, 0, :], 0.0)
        ltmp = psb("ltmp", [128, 2, 8])
        for l in range(1, L):
            P.tt("dve", ltmp[:], lbe[:, :, l, :], lrec[:], ALU.mult)
            P.tt("dve", lbT[:, :, l, :], lbT[:, :, l - 1, :], ltmp[:], ALU.add)
        P.ts("dve", oml[:].rearrange("p a b c -> p (a b c)"), lbT[:].rearrange("p a b c -> p (a b c)"),
             -1.0, 1.0, ALU.mult, ALU.add)
        P.dma("sp", vtmp[0:16, :], gfin_in[:, :])
        ps = pps.next()
        P.tr(ps[:, 0:16], vtmp[0:16, :], ident[0:16, 0:16])
        P.copy("dve", gfT[:], ps[:, 0:16])
        P.dma("sp", vtmp[0:32, :], cc_in[:, :])
        scs = psb("scs", [32, 128])
        P.act(scs[:], vtmp[0:32, :], AF.Silu)
        ps = pps.next()
        P.tr(ps[:, 0:32], scs[:], ident[0:32, 0:32])
        scT = psb("scT", [128, 2, 16])
        P.copy("dve", scT[:].rearrange("p a b -> p (a b)"), ps[:, 0:32])
        wm = Ring(ph, ncw, "wm", 3, [128, DC, 512], F32)
        for l in range(nlayers):
            for cb in range(24):
                wt = wm.next()
                P.dma("sp", wt[:], w_mod[l, :, cb * 512:(cb + 1) * 512].rearrange("(c p) n -> p c n", p=128), key="w")
                ps = pps.next()
                for j in range(4):
                    for c in range(DC):
                        P.mm(ps[:, 2 * j:2 * j + 2], wt[:, c, j * 128:(j + 1) * 128], scT[:, :, c],
                             start=(c == 0), stop=(c == DC - 1))
                for j in range(4):
                    ft = cb * 4 + j
                    P.ts("dve", modT[:, l, ft, :], ps[:, 2 * j:2 * j + 2], vecT[:, l, ft:ft + 1], None, ALU.add)
            for (A, gi0, si) in ((A1, 96, 1), (A2, 112, 4)):
                for r in range(2):
                    P.stt(A[:, l, :, r], modT[:, l, si * 16:(si + 1) * 16, r], 1.0, vecT[:, l, gi0:gi0 + 16],
                          ALU.add, ALU.mult)
    P.barrier()
    if "modT" in dbg:
        o = nc.dram_tensor("o_modT", [128, L * 96 * 2], F32, kind="ExternalOutput").ap()
        P.dma("sp", o[:, :], modT[:].rearrange("p a b c -> p (a b c)"))
        o2 = nc.dram_tensor("o_lbT", [128, 2 * L * 8], F32, kind="ExternalOutput").ap()
        P.dma("sp", o2[:, :], lbT[:].rearrange("p a b c -> p (a b c)"))

    def norm_phase(ph, l, A, shift_i, hT, tile_cb=None):
        xr = Ring(ph, ncw, "nx", 2, [128, DC, 256], F32)
        sqr = Ring(ph, ncw, "nsq", 2, [128, 256], BF16)
        rsr = Ring(ph, ncw, "nrs", 2, [128, 256], F32)
        tmr = Ring(ph, ncw, "ntm", 3, [128, 256], F32)
        for t0 in range(0, T, 256):
            r = 0 if t0 < NL else 1
            xt = xr.next()
            for c in range(DC):
                P.dma("sp", xt[:, c, :], XT[c][:, t0:t0 + 256])
            ps = pps.next()
            for c in range(DC):
                sq = sqr.next()
                P.act(sq[:], xt[:, c, :], AF.Square)
                P.mm(ps[:, 0:256], onesb[:], sq[:], start=(c == 0), stop=(c == DC - 1))
            rs = rsr.next()
            P.ts("dve", rs[:], ps[:, 0:256], 1.0 / D, EPS, ALU.mult, ALU.add)
            P.act(rs[:], rs[:], AF.Sqrt)
            P.op("dve", lambda e: e.reciprocal(out=rs[:], in_=rs[:]), r=[rs[:]], w=[rs[:]])
            for c in range(DC):
                tm = tmr.next()
                P.tt("dve", tm[:], xt[:, c, :], rs[:], ALU.mult)
                if tile_cb is None:
                    P.ts("dve", hT[:, c, t0:t0 + 256], tm[:], A[:, l, c, r:r + 1],
                         modT[:, l, shift_i * 16 + c, r:r + 1], ALU.mult, ALU.add)
                else:
                    tile_cb(t0, c, r, tm)

    if stop_after < 1:
        nlayers = 0

    for l in range(nlayers):
        with ExitStack() as ph:
            hT = ph.enter_context(ncw.sbuf_tensor("hT", [128, DC, T], BF16))
            with ExitStack() as ph2:
                norm_phase(ph2, l, A1, 0, hT)
            P.barrier()
            if l == 0 and "hT" in dbg:
                o = nc.dram_tensor("o_hT", [128, DC * T], BF16, kind="ExternalOutput").ap()
                P.dma("sp", o[:, :], hT[:].rearrange("p a b -> p (a b)"))
            wr = Ring(ph, ncw, "wi", 3, [128, DC, 512], BF16)
            cosT = ph.enter_context(ncw.sbuf_tensor("cosT", [128, T], F32))
            sinT = ph.enter_context(ncw.sbuf_tensor("sinT", [128, T], F32))
            RTb = ph.enter_context(ncw.sbuf_tensor("RTb", [128, 128], BF16))
            P.dma("sp", cosT[:], cin["cosT"][:, :])
            P.dma("sp", sinT[:], cin["sinT"][:, :])
            P.dma("pool", RTb[:], cin["RT"][:, :])
            ev32 = Ring(ph, ncw, "ev32", 3, [128, 512], F32)
            ev16 = Ring(ph, ncw, "ev16", 3, [128, 512], BF16)
            evb = Ring(ph, ncw, "evb", 3, [128, 512], F32)
            for cb in range(NIN // 512):
                wt = wr.next()
                P.dma("pool", wt[:], w_in[l, :, cb * 512:(cb + 1) * 512].rearrange("(c p) n -> p c n", p=128), key="w")
                split = cb // 2 if cb < 18 else 9
                if split in (3, 8):
                    dst = VT if split == 3 else VC
                    c0 = (cb % 2) * 512
                    for tt_ in range(T // 128):
                        ps = pps.next()
                        for c in range(DC):
                            P.mm(ps[:], hT[:, c, tt_ * 128:(tt_ + 1) * 128], wt[:, c, :],
                                 start=(c == 0), stop=(c == DC - 1))
                        o = ev16.next()
                        P.copy("act" if tt_ % 2 else "dve", o[:], ps[:])
                        P.dma("sp", dst[tt_ * 128:(tt_ + 1) * 128, c0:c0 + 512], o[:])
                    continue
                for j in range(4):
                    ft = cb * 4 + j
                    hh = ft % 8
                    for (t0, tn) in TBLK:
                        ps = pps.next()
                        for c in range(DC):
                            P.mm(ps[:, 0:tn], wt[:, c, j * 128:(j + 1) * 128], hT[:, c, t0:t0 + tn],
                                 start=(c == 0), stop=(c == DC - 1))
                        if split in (0, 4):
                            o = ev16.next()
                            P.act(o[:, 0:tn], ps[:, 0:tn], AF.Silu)
                            P.dma("sp", (QA if split == 0 else GA)[hh][:, t0:t0 + tn], o[:, 0:tn])
                        elif split in (1, 2):
                            dr = split - 1
                            s = ev32.next()
                            P.act(s[:, 0:tn], ps[:, 0:tn], AF.Sigmoid)
                            f = evb.next()
                            P.ts("dve", f[:, 0:tn], s[:, 0:tn], oml[:, dr, l, hh:hh + 1], lbT[:, dr, l, hh:hh + 1],
                                 ALU.mult, ALU.add)
                            k = ev16.next()
                            P.ts("dve", k[:, 0:tn], f[:, 0:tn], -1.0, 1.0, ALU.mult, ALU.add)
                            P.dma("sp", (KF if dr == 0 else KB)[hh][:, t0:t0 + tn], k[:, 0:tn])
                            lf = ev32.next()
                            P.act(lf[:, 0:tn], f[:, 0:tn], AF.Ln)
                            P.dma("sp", (LFF if dr == 0 else LFB)[hh][:, t0:t0 + tn], lf[:, 0:tn])
                        elif split == 5:
                            o = ev32.next()
                            P.copy("act", o[:, 0:tn], ps[:, 0:tn])
                            P.dma("sp", UB[hh][:, t0:t0 + tn], o[:, 0:tn])
                        elif split in (6, 7):
                            ub = ev16.next()
                            P.copy("act", ub[:, 0:tn], ps[:, 0:tn])
                            ps2 = pps.next()
                            P.mm(ps2[:, 0:tn], RTb[:], ub[:, 0:tn], start=True, stop=True)
                            t1 = ev32.next()
                            P.tt("dve", t1[:, 0:tn], ub[:, 0:tn], cosT[:, t0:t0 + tn], ALU.mult)
                            t2 = evb.next()
                            P.tt("dve", t2[:, 0:tn], ps2[:, 0:tn], sinT[:, t0:t0 + tn], ALU.mult)
                            o = ev16.next()
                            P.tt("dve", o[:, 0:tn], t1[:, 0:tn], t2[:, 0:tn], ALU.add)
                            P.dma("sp", (QC if split == 6 else KC)[hh][:, t0:t0 + tn], o[:, 0:tn])
                        else:
                            o = ev16.next()
                            P.act(o[:, 0:tn], ps[:, 0:tn], AF.Sigmoid)
                            P.dma("sp", UG[ft - 72][:, t0:t0 + tn], o[:, 0:tn])
        P.barrier()
        if stop_after < 2:
            break

        with ExitStack() as ph:
            def hs(name, shape, dt=F32):
                return ph.enter_context(ncw.sbuf_tensor(name, list(shape), dt))
            maskf = hs("maskf", [CH, CH], I32); maskb = hs("maskb", [CH, CH], I32)
            P.dma("sp", maskf[:], cin["maskf"][:, :]); P.dma("sp", maskb[:], cin["maskb"][:, :])
            onec = hs("onec", [128, 1])
            P.memset("dve", onec[:], 1.0)
            q = hs("hq", [128, T], BF16); kf = hs("hkf", [128, T], BF16); kb = hs("hkb", [128, T], BF16)
            ga = hs("hga", [128, T], BF16)
            lff = hs("hlff", [128, T]); lfb = hs("hlfb", [128, T])
            Bf = hs("hBf", [128, T]); Eb = hs("hEb", [128, T]); Br = hs("hBr", [128, T]); Et = hs("hEt", [128, T])
            qdf = hs("hqdf", [128, T], BF16); kdf = hs("hkdf", [128, T], BF16)
            qdb = hs("hqdb", [128, T], BF16); kdb = hs("hkdb", [128, T], BF16)
            vt = hs("hv", [CH, NCHUNK, 128], BF16)
            oTf = hs("hoTf", [128, T]); oTb = hs("hoTb", [128, T])
            sqo = hs("hsq", [128, T], BF16)
            DF = hs("hDF", [128, NCHUNK]); DB = hs("hDB", [128, NCHUNK]); dtmp = hs("hdtmp", [128, NCHUNK])
            Sf = hs("hSf", [128, 128]); Sb = hs("hSb", [128, 128])
            Sfb = Ring(ph, ncw, "hSfb", 2, [128, 128], BF16); Sbb = Ring(ph, ncw, "hSbb", 2, [128, 128], BF16)
            Atf = Ring(ph, ncw, "hAf", 3, [CH, CH], BF16); Atb = Ring(ph, ncw, "hAb", 3, [CH, CH], BF16)
            for rr in (Atf, Atb):
                for t_ in rr.t:
                    P.memset("dve", t_[:], 0.0)
            kdt = Ring(ph, ncw, "hkdt", 4, [CH, 128], BF16)
            stmp = Ring(ph, ncw, "hstmp", 4, [128, 128], F32)
            yr = Ring(ph, ncw, "hy", 2, [128, 512], F32); yo = Ring(ph, ncw, "hyo", 2, [128, 512], BF16)
            rsr = Ring(ph, ncw, "hrs", 2, [128, 512], F32)

            def v3(ap):
                return ap.rearrange("p (n s) -> p n s", s=CH)

            forder = [32, 33, 34, 35] + list(range(32))
            border = list(range(35, -1, -1))
            for h in range(8):
                P.dma("sp", q[:], QA[h][:, :]); P.dma("sp", kf[:], KF[h][:, :]); P.dma("sp", kb[:], KB[h][:, :])
                P.dma("sp", lff[:], LFF[h][:, :]); P.dma("sp", lfb[:], LFB[h][:, :]); P.dma("sp", ga[:], GA[h][:, :])
                P.dma("sp", vt[:], VT[:, h * 128:(h + 1) * 128].rearrange("(n s) v -> s n v", s=CH))
                P.op("dve", lambda e: e.tensor_tensor_scan(out=Bf[:, NL:T], data0=onec[:, 0:1].to_broadcast([128, NCX]),
                                                           data1=lff[:, NL:T], initial=0.0, op0=ALU.mult, op1=ALU.add),
                     r=[lff[:], onec[:]], w=[Bf[:]])
                P.op("dve", lambda e: e.tensor_tensor_scan(out=Bf[:, 0:NL], data0=onec[:, 0:1].to_broadcast([128, NL]),
                                                           data1=lff[:, 0:NL], initial=Bf[:, T - 1:T], op0=ALU.mult, op1=ALU.add),
                     r=[lff[:], onec[:], Bf[:]], w=[Bf[:]])
                P.op("dve", lambda e: e.tensor_tensor_scan(out=Eb[:, :], data0=onec[:, 0:1].to_broadcast([128, T]),
                                                           data1=lfb[:, :], initial=0.0, op0=ALU.mult, op1=ALU.add),
                     r=[lfb[:], onec[:]], w=[Eb[:]])
                P.tt("dve", Eb[:], Eb[:], lfb[:], ALU.subtract)
                Bfm = v3(Bf[:])[:, :, CH // 2]
                Ebm = v3(Eb[:])[:, :, CH // 2]
                P.tt("dve", dtmp[:, 0:NCHUNK - 1], Bfm[:, 1:NCHUNK], Bfm[:, 0:NCHUNK - 1], ALU.subtract)
                P.tt("dve", dtmp[:, NCHUNK - 1:NCHUNK], Bfm[:, 0:1], Bfm[:, NCHUNK - 1:NCHUNK], ALU.subtract)
                P.act(DF[:], dtmp[:], AF.Exp)
                P.tt("dve", dtmp[:, 1:NCHUNK], Ebm[:, 1:NCHUNK], Ebm[:, 0:NCHUNK - 1], ALU.subtract)
                P.act(DB[:, 1:NCHUNK], dtmp[:, 1:NCHUNK], AF.Exp)
                P.tt("dve", v3(Br[:]), v3(Bf[:]), Bfm.unsqueeze(2).to_broadcast([128, NCHUNK, CH]), ALU.subtract)
                P.act(Et[:], Br[:], AF.Exp)
                P.tt("dve", qdf[:], q[:], Et[:], ALU.mult)
                P.act(Et[:], Br[:], AF.Exp, scale=-1.0)
                P.tt("dve", kdf[:], kf[:], Et[:], ALU.mult)
                P.tt("dve", v3(Br[:]), v3(Eb[:]), Ebm.unsqueeze(2).to_broadcast([128, NCHUNK, CH]), ALU.subtract)
                P.act(Et[:], Br[:], AF.Exp)
                P.tt("dve", kdb[:], kb[:], Et[:], ALU.mult)
                P.act(Et[:], Br[:], AF.Exp, scale=-1.0)
                P.tt("dve", qdb[:], q[:], Et[:], ALU.mult)
                P.memset("dve", Sf[:], 0.0); P.memset("dve", Sb[:], 0.0)
                sfb = Sfb.next(); sbb = Sbb.next()
                P.memset("dve", sfb[:], 0.0); P.memset("dve", sbb[:], 0.0)
                cur = {"f": sfb, "b": sbb}
                for i in range(NCHUNK):
                    for dr_ in ("f", "b"):
                        n = forder[i] if dr_ == "f" else border[i]
                        qd, kd, msk, S, Sring, Ar, oT = ((qdf, kdf, maskf, Sf, Sfb, Atf, oTf) if dr_ == "f"
                                                         else (qdb, kdb, maskb, Sb, Sbb, Atb, oTb))
                        cs = slice(n * CH, (n + 1) * CH)
                        ps_s = pps.next()
                        P.mm(ps_s[0:CH, 0:CH], kd[:, cs], qd[:, cs], True, True)
                        A = Ar.next()
                        P.op("dve", lambda e: e.copy_predicated(out=A[:], mask=msk[:], data=ps_s[0:CH, 0:CH]),
                             r=[msk[:], ps_s[:]], w=[A[:]])
                        ps_k = pps.next()
                        pkb = ps_k[:].bitcast(BF16)
                        P.tr(pkb[0:CH, 0:128], kd[:, cs], identb[:])
                        kt = kdt.next()
                        P.copy("act", kt[:], pkb[0:CH, 0:128])
                        ps_o = pacc.next()
                        P.mm(ps_o[:, 0:CH], vt[:, n, :], A[:], True, False)
                        P.mm(ps_o[:, 0:CH], cur[dr_][:], qd[:, cs], False, True)
                        P.copy("act", oT[:, cs], ps_o[:, 0:CH])
                        if i < NCHUNK - 1:
                            ps_p = pacc.next()
                            P.mm(ps_p[:, 0:128], kt[:], vt[:, n, :], True, True)
                            tmp = stmp.next()
                            P.tt("dve", tmp[:], S[:], ps_p[:, 0:128], ALU.add)
                            dcol = DF[:, n:n + 1] if dr_ == "f" else DB[:, n:n + 1]
                            P.ts("dve", S[:], tmp[:], dcol, None, ALU.mult)
                            nb = Sring.next()
                            P.act(nb[:], tmp[:], AF.Copy, scale=dcol)
                            cur[dr_] = nb
                P.tt("dve", oTf[:], oTf[:], oTb[:], ALU.add)
                P.act(sqo[:], oTf[:], AF.Square)
                for (t0, tn) in TBLK:
                    ps = pps.next()
                    P.mm(ps[:, 0:tn], onesb[:], sqo[:, t0:t0 + tn], True, True)
                    rs = rsr.next()
                    P.ts("dve", rs[:, 0:tn], ps[:, 0:tn], 1.0 / 128, EPS, ALU.mult, ALU.add)
                    P.act(rs[:, 0:tn], rs[:, 0:tn], AF.Sqrt)
                    P.op("dve", lambda e: e.reciprocal(out=rs[:, 0:tn], in_=rs[:, 0:tn]), r=[rs[:]], w=[rs[:]])
                    y1 = yr.next()
                    P.tt("dve", y1[:, 0:tn], oTf[:, t0:t0 + tn], rs[:, 0:tn], ALU.mult)
                    y2 = yo.next()
                    P.stt(y2[:, 0:tn], y1[:, 0:tn], vecT[:, l, 128 + h:129 + h], ga[:, t0:t0 + tn], ALU.mult, ALU.mult)
                    P.dma("sp", YA[h][:, t0:t0 + tn], y2[:, 0:tn])
        P.barrier()
        if stop_after < 3:
            break
        with ExitStack() as ph:
            def hs(name, shape, dt=F32):
                return ph.enter_context(ncw.sbuf_tensor(name, list(shape), dt))
            wp = hs("wp", [128, 4, 2, 256], BF16)
            P.dma("pool", wp[:], w_pool[l].rearrange("g (c p) e -> p g c e", p=128), key="w")
            PADT = T + 64
            ua = Ring(ph, ncw, "pu", 2, [128, PADT], F32)
            sa = hs("psa", [128, PADT]); sbb_ = hs("psb", [128, PADT])
            inv = hs("pinv", [128, T])
            dT = hs("pdT", [128, 2, T], BF16)
            dtm = hs("pdtm", [128, T])
            yo = Ring(ph, ncw, "pyo", 2, [128, 512], BF16)
            segs = ((16, 0, NL), (NL + 48, NL, NCX))
            for gi, w_ in enumerate((2, 4, 8, 16)):
                P.dma("sp", inv[:], cin["invcnt"][gi])
                for cc in range(2):
                    hh = gi * 2 + cc
                    u = ua.next()
                    P.memset("dve", u[:], 0.0)
                    for (o, t0, n) in segs:
                        P.dma("sp", u[:, o:o + n], UB[hh][:, t0:t0 + n])
                    for (o, t0, n) in segs:
                        lo, hi = o - 8, o + n + 8
                        P.tt("dve", sa[:, lo:hi], u[:, lo - 1:hi - 1], u[:, lo:hi], ALU.add)
                        src = sa
                        if w_ >= 4:
                            P.tt("dve", sbb_[:, lo + 2:hi - 2], sa[:, lo + 1:hi - 3], sa[:, lo + 3:hi - 1], ALU.add)
                            src = sbb_
                        if w_ >= 8:
                            P.tt("dve", sa[:, lo + 4:hi - 4], sbb_[:, lo + 2:hi - 6], sbb_[:, lo + 6:hi - 2], ALU.add)
                            src = sa
                        if w_ >= 16:
                            P.tt("dve", sbb_[:, o:o + n], sa[:, o - 4:o + n - 4], sa[:, o + 4:o + n + 4], ALU.add)
                            src = sbb_
                        P.tt("dve", dtm[:, t0:t0 + n], src[:, o:o + n], inv[:, t0:t0 + n], ALU.mult)
                        P.tt("dve", dT[:, cc, t0:t0 + n], dtm[:, t0:t0 + n], u[:, o:o + n], ALU.subtract)
                for et in range(2):
                    hh = gi * 2 + et
                    for (t0, tn) in TBLK:
                        ps = pps.next()
                        for cc in range(2):
                            P.mm(ps[:, 0:tn], wp[:, gi, cc, et * 128:(et + 1) * 128], dT[:, cc, t0:t0 + tn],
                                 cc == 0, cc == 1)
                        o_ = yo.next()
                        P.act(o_[:, 0:tn], ps[:, 0:tn], AF.Copy, scale=vecT[:, l, 136 + hh:137 + hh])
                        P.dma("sp", YB[hh][:, t0:t0 + tn], o_[:, 0:tn])
        P.barrier()
        if stop_after < 4:
            break
        with ExitStack() as ph:
            def hs(name, shape, dt=F32):
                return ph.enter_context(ncw.sbuf_tensor(name, list(shape), dt))
            am = hs("am", [128, 14, 256])
            P.dma("sp", am[:], cin["attmask"][:, :, :])
            qT = hs("aq", [128, T], BF16); kT = hs("ak", [128, T], BF16)
            vv = hs("av", [128, T // 128, 128], BF16)
            BT = hs("aBT", [128, 14, 256])
            sc_ = Ring(ph, ncw, "asc", 3, [128, 256], F32)
            pTr = Ring(ph, ncw, "apT", 4, [128, 256], BF16)
            rec = Ring(ph, ncw, "arec", 2, [128, 256], F32)
            yo = Ring(ph, ncw, "ayo", 2, [128, 256], BF16)
            SCALE = 128.0 ** -0.5
            for h in range(8):
                P.dma("sp", qT[:], QC[h][:, :]); P.dma("sp", kT[:], KC[h][:, :])
                P.dma("sp", vv[:], VC[:, h * 128:(h + 1) * 128].rearrange("(n p) v -> p n v", p=128))
                P.dma("sp", BT[:], rpbg[l, h])
                P.tt("dve", BT[:], BT[:], am[:], ALU.add)
                for qb in range(9):
                    q0 = qb * 256
                    if qb == 0:
                        kts = [(128 * j, 6 + j) for j in range(4)]
                    elif qb == 7:
                        kts = [((24 + 2 * j) * 64, 10 + j) for j in range(4)]
                    elif qb == 8:
                        kts = []
                    else:
                        kts = [((4 * qb - 4 + 2 * j) * 64, j) for j in range(6)]
                    kts = kts + [(NL, None), (NL + 128, None)]
                    ps_o = pacc.next(); ps_d = pacc.next()
                    for i, (ks, bi) in enumerate(kts):
                        ps_s = pps.next()
                        P.mm(ps_s[:, 0:256], kT[:, ks:ks + 128], qT[:, q0:q0 + 256], True, True)
                        pT = pTr.next()
                        if bi is not None:
                            t_ = sc_.next()
                            P.stt(t_[:], ps_s[:, 0:256], SCALE, BT[:, bi, :], ALU.mult, ALU.add)
                            P.act(pT[:], t_[:], AF.Exp)
                        else:
                            P.act(pT[:], ps_s[:, 0:256], AF.Exp, scale=SCALE)
                        P.mm(ps_o[:, 0:256], vv[:, ks // 128, :], pT[:], i == 0, i == len(kts) - 1)
                        P.mm(ps_d[:, 0:256], onesb[:], pT[:], i == 0, i == len(kts) - 1)
                    rc = rec.next()
                    P.op("dve", lambda e: e.reciprocal(out=rc[:], in_=ps_d[:, 0:256]), r=[ps_d[:]], w=[rc[:]])
                    o_ = yo.next()
                    P.tt("dve", o_[:], ps_o[:, 0:256], rc[:], ALU.mult)
                    P.dma("sp", YC[h][:, q0:q0 + 256], o_[:])
        P.barrier()
        if stop_after < 5:
            break

        with ExitStack() as ph:
            wb = ph.enter_context(ncw.sbuf_tensor("wb", [128, 3, 8, D], BF16))
            for j in range(3):
                for hf in range(2):
                    P.dma("pool", wb[:, j, hf * 4:(hf + 1) * 4, :],
                          w_branch[l, j, hf * 512:(hf + 1) * 512, :].rearrange("(c p) d -> p c d", p=128), key="w")
            yr3 = [Ring(ph, ncw, f"my{j}", 2, [128, 8, 512], BF16) for j in range(3)]
            gr = Ring(ph, ncw, "mg_g", 4, [128, 512], BF16)
            ar = Ring(ph, ncw, "mg_a", 2, [128, 512], F32)
            t2r = Ring(ph, ncw, "mg_t", 2, [128, 512], F32)
            mo = Ring(ph, ncw, "mg_o", 2, [128, 512], BF16)
            gr8 = Ring(ph, ncw, "mg_g8", 8, [128, 512], BF16)
            its = [(bi, dt_) for bi in range(len(TBLK)) for dt_ in range(DC)]
            ystate = {}

            def m_loads(it):
                bi, dt_ = it
                t0, tn = TBLK[bi]
                if dt_ == 0:
                    ys = [rr.next() for rr in yr3]
                    for j, Y in enumerate((YA, YB, YC)):
                        for c in range(8):
                            P.dma("sp", ys[j][:, c, 0:tn], Y[c][:, t0:t0 + tn])
                    ystate[bi] = ys
                gs = []
                for j in range(3):
                    g = gr8.next()
                    P.dma("sp", g[:, 0:tn], UG[j * 16 + dt_][:, t0:t0 + tn])
                    gs.append(g)
                return gs

            pend = m_loads(its[0])
            for ii, (bi, dt_) in enumerate(its):
                t0, tn = TBLK[bi]
                gs = pend
                if ii + 1 < len(its):
                    pend = m_loads(its[ii + 1])
                ys = ystate[bi]
                acc = ar.next()
                for j in range(3):
                    g = gs[j]
                    ps = pall.next()
                    for c in range(8):
                        P.mm(ps[:, 0:tn], wb[:, j, c, dt_ * 128:(dt_ + 1) * 128], ys[j][:, c, 0:tn], c == 0, c == 7)
                    if j == 0:
                        P.tt("dve", acc[:, 0:tn], ps[:, 0:tn], g[:, 0:tn], ALU.mult)
                    else:
                        t2 = t2r.next()
                        P.tt("dve", t2[:, 0:tn], ps[:, 0:tn], g[:, 0:tn], ALU.mult)
                        if j == 1:
                            P.tt("pool", acc[:, 0:tn], acc[:, 0:tn], t2[:, 0:tn], ALU.add)
                        else:
                            o_ = mo.next()
                            P.tt("pool", o_[:, 0:tn], acc[:, 0:tn], t2[:, 0:tn], ALU.add)
                            P.dma("sp", MG[dt_][:, t0:t0 + tn], o_[:, 0:tn])
        P.barrier()
        with ExitStack() as ph:
            wo = ph.enter_context(ncw.sbuf_tensor("wo", [128, DC, D], BF16))
            for qd_ in range(4):
                P.dma("pool", wo[:, qd_ * 4:(qd_ + 1) * 4, :],
                      w_out[l, qd_ * 512:(qd_ + 1) * 512, :].rearrange("(c p) d -> p c d", p=128), key="w")
            mgr = Ring(ph, ncw, "wo_m", 2, [128, DC, 512], BF16)
            xr = Ring(ph, ncw, "wo_x", 3, [128, 512], F32)
            xo = Ring(ph, ncw, "wo_o", 3, [128, 512], F32)
            xr = Ring(ph, ncw, "wo_x4", 5, [128, 512], F32)
            its = [(bi, et) for bi in range(len(TBLK)) for et in range(DC)]
            mstate = {}

            def w_loads(it):
                bi, et = it
                t0, tn = TBLK[bi]
                if et == 0:
                    mg = mgr.next()
                    for c in range(DC):
                        P.dma("sp", mg[:, c, 0:tn], MG[c][:, t0:t0 + tn])
                    mstate[bi] = mg
                xt = xr.next()
                P.dma("sp", xt[:, 0:tn], XT[et][:, t0:t0 + tn])
                return xt

            pend = [w_loads(its[0]), w_loads(its[1])]
            for ii, (bi, et) in enumerate(its):
                t0, tn = TBLK[bi]
                r = 0 if t0 < NL else 1
                xt = pend.pop(0)
                if ii + 2 < len(its):
                    pend.append(w_loads(its[ii + 2]))
                mg = mstate[bi]
                ps = pall.next()
                for c in range(DC):
                    P.mm(ps[:, 0:tn], wo[:, c, et * 128:(et + 1) * 128], mg[:, c, 0:tn], c == 0, c == DC - 1)
                o_ = xo.next()
                P.stt(o_[:, 0:tn], ps[:, 0:tn], modT[:, l, 32 + et, r:r + 1], xt[:, 0:tn], ALU.mult, ALU.add)
                P.dma("sp", XT[et][:, t0:t0 + tn], o_[:, 0:tn])
        P.barrier()
        if stop_after < 6:
            break
        with ExitStack() as ph:
            def hs(name, shape, dt=F32):
                return ph.enter_context(ncw.sbuf_tensor(name, list(shape), dt))
            posm = hs("posm", [NE, T]); gw = hs("gwT", [NE, T])
            posmt = hs("posmt", [128, T // 128, NE])
            wrt = hs("wrt", [128, DC, NE])
            iotar = hs("iotar", [128, NS])
            P.dma("sp", iotar[:], cin["iotar"][:, :])
            P.dma("sp", wrt[:], w_router[l].rearrange("(c p) e -> p c e", p=128))
            m8 = hs("m8", [NE, 8]); thr = hs("thr", [NE, 2]); onee = hs("onee", [NE, 1])
            hk = ExitStack()
            h2tok = hk.enter_context(ncw.sbuf_tensor("h2tok", [128, T // 128, D], BF16))
            pk = ExitStack()
            affT = pk.enter_context(ncw.sbuf_tensor("affT", [NE, T], F32))
            wrk = pk.enter_context(ncw.sbuf_tensor("awrk", [NE, T], F32))
            mk = pk.enter_context(ncw.sbuf_tensor("mkT", [NE, T], F32))
            with ExitStack() as ph2:
                h2f = Ring(ph2, ncw, "h2f", 3, [128, 256], F32)
                h2b = Ring(ph2, ncw, "h2b", 3, [128, 256], BF16)
                sm = Ring(ph2, ncw, "rsm", 2, [128, 2, NE], F32)
                sm1 = Ring(ph2, ncw, "rsm1", 6, [128, 2], F32)
                state = {}

                def cb(t0, c, r, tm):
                    if c == 0:
                        state["ps"] = [pacc.next(), pacc.next()]
                    psr2 = state["ps"]
                    hf = h2f.next()
                    P.ts("dve", hf[:], tm[:], A2[:, l, c, r:r + 1], modT[:, l, 3 * 16 + c, r:r + 1], ALU.mult, ALU.add)
                    for s_ in range(2):
                        P.mm(psr2[s_][:, 0:NE], hf[:, s_ * 128:(s_ + 1) * 128], wrt[:, c, :],
                             c == 0, c == DC - 1)
                    hb = h2b.next()
                    P.copy("act", hb[:], hf[:])
                    pt = pps.next()
                    ptb = pt[:].bitcast(BF16)
                    for s_ in range(2):
                        P.tr(ptb[:, s_ * 128:(s_ + 1) * 128], hb[:, s_ * 128:(s_ + 1) * 128], identb[:])
                    tt0 = t0 // 128
                    P.copy("act", h2tok[:, tt0:tt0 + 2, c * 128:(c + 1) * 128],
                           ptb[:, 0:256].rearrange("p (s d) -> p s d", s=2))
                    if c == DC - 1:
                        mx = sm1.next()
                        for s_ in range(2):
                            P.op("dve", lambda e: e.tensor_reduce(out=mx[:, s_:s_ + 1], in_=psr2[s_][:, 0:NE],
                                                                  axis=mybir.AxisListType.X, op=ALU.max),
                                 r=[psr2[s_][:]], w=[mx[:]])
                        P.ts("dve", mx[:], mx[:], -1.0, None, ALU.mult)
                        ex = sm.next()
                        for s_ in range(2):
                            P.act(ex[:, s_, :], psr2[s_][:, 0:NE], AF.Exp, bias=mx[:, s_:s_ + 1])
                        su = sm1.next()
                        P.op("dve", lambda e: e.tensor_reduce(out=su[:], in_=ex[:], axis=mybir.AxisListType.X, op=ALU.add),
                             r=[ex[:]], w=[su[:]])
                        P.op("dve", lambda e: e.reciprocal(out=su[:], in_=su[:]), r=[su[:]], w=[su[:]])
                        for s_ in range(2):
                            P.ts("dve", ex[:, s_, :], ex[:, s_, :], su[:, s_:s_ + 1], None, ALU.mult)
                            pa = pps.next()
                            P.tr(pa[0:NE, 0:128], ex[:, s_, :], ident[:])
                            P.copy("act", affT[:, t0 + s_ * 128:t0 + (s_ + 1) * 128], pa[0:NE, 0:128])

                norm_phase(ph2, l, A2, 3, None, tile_cb=cb)
            P.barrier()
            P.memset("dve", onee[:], 1.0)
            P.copy("dve", wrk[:], affT[:])
            for si, (a, b, rounds) in enumerate(((0, NL, CAPL // 8), (NL, T, CAPC // 8))):
                for _ in range(rounds):
                    P.op("dve", lambda e: e.max(out=m8[:], in_=wrk[:, a:b]), r=[wrk[:]], w=[m8[:]])
                    P.op("dve", lambda e: e.match_replace(out=wrk[:, a:b], in_to_replace=m8[:], in_values=wrk[:, a:b],
                                                          imm_value=-1.0), r=[m8[:], wrk[:]], w=[wrk[:]])
                P.copy("dve", thr[:, si:si + 1], m8[:, 7:8])
                P.ts("dve", mk[:, a:b], affT[:, a:b], thr[:, si:si + 1], None, ALU.is_ge)
                P.op("dve", lambda e: e.tensor_tensor_scan(out=posm[:, a:b], data0=onee[:, 0:1].to_broadcast([NE, b - a]),
                                                           data1=mk[:, a:b], initial=float(0 if si == 0 else CAPL),
                                                           op0=ALU.mult, op1=ALU.add),
                     r=[mk[:], onee[:]], w=[posm[:]])
            P.tt("dve", gw[:], mk[:], affT[:], ALU.mult)
            P.tt("dve", posm[:], posm[:], mk[:], ALU.mult)
            P.ts("dve", posm[:], posm[:], -1.0, None, ALU.add)
            for tt_ in range(T // 128):
                pa = pps.next()
                P.tr(pa[:, 0:NE], posm[:, tt_ * 128:(tt_ + 1) * 128], ident[0:NE, 0:NE])
                P.copy("act", posmt[:, tt_, :], pa[:, 0:NE])
            if l == 0 and "affT" in dbg:
                o = nc.dram_tensor("o_affT", [NE, T], F32, kind="ExternalOutput").ap()
                P.dma("sp", o[:, :], affT[:])
                o = nc.dram_tensor("o_posm", [NE, T], F32, kind="ExternalOutput").ap()
                P.dma("sp", o[:, :], posm[:])
            P.barrier()
            pk.close()
            with ExitStack() as ph2:
                Per = Ring(ph2, ncw, "Pe", 1, [128, T // 128, NS], BF16)
                xgr = Ring(ph2, ncw, "xg", 1, [128, DC, NS], BF16)
                hdr = Ring(ph2, ncw, "hid", 1, [128, 8, NS], BF16)
                wq = Ring(ph2, ncw, "wq", 4, [128, DC, 512], BF16)
                sg = Ring(ph2, ncw, "sg", 2, [128, NS], F32)
                yt_ = Ring(ph2, ncw, "yt", 3, [128, 512], BF16)
                for e_ in range(NE):
                    Pe = Per.next()
                    for tt_ in range(T // 128):
                        a, b = (0, CAPL) if tt_ < 16 else (CAPL, NS)
                        P.ts("dve", Pe[:, tt_, a:b], iotar[:, a:b],
                             posmt[:, tt_, e_:e_ + 1], None, ALU.is_equal)
                    xg = xgr.next()
                    for dt_ in range(DC):
                        ps = pall.next()
                        for tt_ in range(16):
                            P.mm(ps[:, 0:CAPL], h2tok[:, tt_, dt_ * 128:(dt_ + 1) * 128], Pe[:, tt_, 0:CAPL],
                                 tt_ == 0, tt_ == 15)
                        for tt_ in range(16, 18):
                            P.mm(ps[:, CAPL:NS], h2tok[:, tt_, dt_ * 128:(dt_ + 1) * 128], Pe[:, tt_, CAPL:NS],
                                 tt_ == 16, tt_ == 17)
                        P.copy("act" if dt_ % 2 else "dve", xg[:, dt_, :], ps[:, 0:NS])
                    hid = hdr.next()
                    for hf in range(2):
                        wg_ = wq.next()
                        P.dma("pool", wg_[:], w_gate[l, e_, :, hf * 512:(hf + 1) * 512].rearrange("(c p) f -> p c f", p=128), key="w")
                        wu_ = wq.next()
                        P.dma("pool", wu_[:], w_up[l, e_, :, hf * 512:(hf + 1) * 512].rearrange("(c p) f -> p c f", p=128), key="w")
                        for j in range(4):
                            ft = hf * 4 + j
                            pg = pall.next(); pu = pall.next()
                            for c in range(DC):
                                P.mm(pg[:, 0:NS], wg_[:, c, j * 128:(j + 1) * 128], xg[:, c, :], c == 0, c == DC - 1)
                            for c in range(DC):
                                P.mm(pu[:, 0:NS], wu_[:, c, j * 128:(j + 1) * 128], xg[:, c, :], c == 0, c == DC - 1)
                            s_ = sg.next()
                            P.act(s_[:], pg[:, 0:NS], AF.Silu)
                            P.tt("dve", hid[:, ft, :], s_[:], pu[:, 0:NS], ALU.mult)
                    for db in range(4):
                        wd_ = wq.next()
                        P.dma("pool", wd_[:, 0:8, :], w_down[l, e_, :, db * 512:(db + 1) * 512].rearrange("(c p) d -> p c d", p=128), key="w")
                        for ct, (c0, cn) in enumerate(((0, 128), (128, 128), (256, 32))):
                            ps = pall.next()
                            for f_ in range(8):
                                P.mm(ps[0:cn, :], hid[:, f_, c0:c0 + cn], wd_[:, f_, :], f_ == 0, f_ == 7)
                            y_ = yt_.next()
                            P.copy("act" if ct % 2 else "dve", y_[0:cn, :], ps[0:cn, :])
                            for i in range(4):
                                P.dma("sp", YM[db * 4 + i][0:cn, (e_ * 3 + ct) * 128:(e_ * 3 + ct + 1) * 128],
                                      y_[0:cn, i * 128:(i + 1) * 128])
            P.barrier()
            hk.close()
            with ExitStack() as ph2:
                iotac = ph2.enter_context(ncw.sbuf_tensor("iotac", [128, 3], F32))
                sel = ph2.enter_context(ncw.sbuf_tensor("sel", [NE, NE, 128], F32))
                P.dma("sp", iotac[:], cin["iotac"][:, :])
                P.dma("sp", sel[:], cin["sel"].rearrange("e k m -> k e m"))
                PG = ph2.enter_context(ncw.sbuf_tensor("PG", [128, NE, 2, 1536], BF16))
                gwb = Ring(ph2, ncw, "gwb", 2, [128, 512], F32)
                ymr = Ring(ph2, ncw, "ym", 3, [128, NE * 3 * 128], BF16)
                xr = Ring(ph2, ncw, "sx", 5, [128, 512], F32)
                xo = Ring(ph2, ncw, "so", 3, [128, 512], F32)
                for grp in ((TBLK[0:3], TBLK[3:5]) if stop_after >= 9 else ()):
                    info = []
                    off = 0
                    for (t0, tn) in grp:
                        r = 0 if t0 < NL else 1
                        cts = ((0, 0, 128), (1, 1, 128)) if r == 0 else ((0, 2, 32),)
                        info.append((t0, tn, r, cts, off))
                        for e_ in range(NE):
                            p1 = pps.next(); p2 = pps.next()
                            P.mm(p1[:, 0:tn], sel[:, e_, :], posm[:, t0:t0 + tn], True, True)
                            P.mm(p2[:, 0:tn], sel[:, e_, :], gw[:, t0:t0 + tn], True, True)
                            g_ = gwb.next()
                            P.copy("act", g_[:, 0:tn], p2[:, 0:tn])
                            for (slot, ct, kk) in cts:
                                P.stt(PG[0:kk, e_, slot, off:off + tn], p1[0:kk, 0:tn], iotac[0:kk, ct:ct + 1],
                                      g_[0:kk, 0:tn], ALU.is_equal, ALU.mult)
                        off += tn
                    its = [(dt_, k) for dt_ in range(DC) for k in range(len(info))]
                    ystate = {}

                    def s_loads(it):
                        dt_, k = it
                        t0, tn = info[k][0], info[k][1]
                        if k == 0:
                            ym = ymr.next()
                            P.dma("sp", ym[:], YM[dt_][:, :])
                            ystate[dt_] = ym
                        xt = xr.next()
                        P.dma("sp", xt[:, 0:tn], XT[dt_][:, t0:t0 + tn])
                        return xt

                    pend = [s_loads(its[0]), s_loads(its[1])]
                    for ii, (dt_, k) in enumerate(its):
                        (t0, tn, r, cts, off) = info[k]
                        xt = pend.pop(0)
                        if ii + 2 < len(its):
                            pend.append(s_loads(its[ii + 2]))
                        ym = ystate[dt_]
                        ps = pacc.next()
                        n_mm = NE * len(cts)
                        i_mm = 0
                        for e_ in range(NE):
                            for (slot, ct, kk) in cts:
                                P.mm(ps[:, 0:tn], ym[0:kk, (e_ * 3 + ct) * 128:(e_ * 3 + ct + 1) * 128],
                                     PG[0:kk, e_, slot, off:off + tn], i_mm == 0, i_mm == n_mm - 1)
                                i_mm += 1
                        o_ = xo.next()
                        P.stt(o_[:, 0:tn], ps[:, 0:tn], modT[:, l, 5 * 16 + dt_, r:r + 1], xt[:, 0:tn],
                              ALU.mult, ALU.add)
                        P.dma("sp", XT[dt_][:, t0:t0 + tn], o_[:, 0:tn])
        P.barrier()

    if stop_after >= 9:
        with ExitStack() as ph:
            xr = Ring(ph, ncw, "fx", 2, [128, DC, 256], F32)
            sqr = Ring(ph, ncw, "fsq", 2, [128, 256], BF16)
            rsr = Ring(ph, ncw, "frs", 2, [128, 256], F32)
            tmr = Ring(ph, ncw, "ftm", 3, [128, 256], F32)
            otr = Ring(ph, ncw, "fo", 2, [128, 2, D], F32)
            for t0 in range(0, NL, 256):
                xt = xr.next()
                for c in range(DC):
                    P.dma("sp", xt[:, c, :], XT[c][:, t0:t0 + 256])
                ps = pacc.next()
                for c in range(DC):
                    sq = sqr.next()
                    P.act(sq[:], xt[:, c, :], AF.Square)
                    P.mm(ps[:, 0:256], onesb[:], sq[:], c == 0, c == DC - 1)
                rs = rsr.next()
                P.ts("dve", rs[:], ps[:, 0:256], 1.0 / D, EPS, ALU.mult, ALU.add)
                P.act(rs[:], rs[:], AF.Sqrt)
                P.op("dve", lambda e: e.reciprocal(out=rs[:], in_=rs[:]), r=[rs[:]], w=[rs[:]])
                ot = otr.next()
                for c in range(DC):
                    tm = tmr.next()
                    P.stt(tm[:], xt[:, c, :], gfT[:, c:c + 1], rs[:], ALU.mult, ALU.mult)
                    pt = pps.next()
                    for s_ in range(2):
                        P.tr(pt[:, s_ * 128:(s_ + 1) * 128], tm[:, s_ * 128:(s_ + 1) * 128], ident[:])
                    P.copy("act" if c % 2 else "dve", ot[:, :, c * 128:(c + 1) * 128],
                           pt[:, 0:256].rearrange("p (s d) -> p s d", s=2))
                for s_ in range(2):
                    P.dma("sp", y_out[t0 + s_ * 128:t0 + (s_ + 1) * 128, :], ot[:, s_, :])

    P.barrier()
    st.close()
    return nc, cst


def _host_inputs(inputs, nlayers=L, ncores=8):
    f = lambda a: np.ascontiguousarray(np.asarray(a, dtype=np.float32))
    x = f(inputs["x"]); c = f(inputs["c"]); ctx = f(inputs["ctx"]); c_ctx = f(inputs["c_ctx"])
    _, dri, dci = _att_tables()
    rpb = f(inputs["rpb"])
    g = rpb[:, :, dri, dci]
    rpbg = np.ascontiguousarray(g.transpose(0, 1, 3, 2, 4))
    vecs = np.concatenate([
        f(inputs["b_mod"]).reshape(L, 96, 128),
        f(inputs["g_norm1"]).reshape(L, 16, 128),
        f(inputs["g_norm2"]).reshape(L, 16, 128),
        f(inputs["g_hgrn"]).reshape(L, 8, 128),
        f(inputs["pool_scale"]).reshape(L, 8, 128)], axis=1)
    shared = {
        "w_mod": f(inputs["w_mod"]), "vecs": np.ascontiguousarray(vecs),
        "lbp": f(inputs["lb_param"]).reshape(64, 128), "gfin": f(inputs["g_final"]).reshape(16, 128),
        "w_in": f(inputs["w_in"]), "w_pool": f(inputs["w_pool"]), "rpbg": rpbg,
        "w_branch": f(inputs["w_branch"]), "w_out": f(inputs["w_out"]), "w_router": f(inputs["w_router"]),
        "w_gate_e": f(inputs["w_gate_e"]), "w_up_e": f(inputs["w_up_e"]), "w_down_e": f(inputs["w_down_e"]),
    }
    for k in ("w_mod", "w_in", "w_pool", "rpbg", "w_branch", "w_out", "w_router", "w_gate_e", "w_up_e", "w_down_e"):
        shared[k] = shared[k][:nlayers]
    maps = []
    for core in range(ncores):
        b = core % 4
        m = dict(shared)
        m["x"] = x[b]
        m["ctx"] = ctx[b]
        m["cc"] = np.concatenate([c[b].reshape(16, 128), c_ctx.reshape(16, 128)], axis=0)
        maps.append(m)
    return maps


def kernel(**inputs):
    nc, cst = build()
    maps = _host_inputs(inputs)
    for m in maps:
        for k, v in cst.items():
            m["k_" + k] = v
    res = run_bass_kernel_spmd(nc, maps, core_ids=list(range(8)))
    return np.stack([res.results[b]["y"] for b in range(4)], axis=0).astype(np.float32)
```

```python
import numpy as np
from contextlib import ExitStack
import concourse.bass as bass
import concourse.mybir as mybir
from concourse.bass_utils import run_bass_kernel_spmd

F32 = mybir.dt.float32
BF16 = mybir.dt.bfloat16
I32 = mybir.dt.int32
AF = mybir.ActivationFunctionType
ALU = mybir.AluOpType

D = 2048
NL = 2048
NCX = 256
T = NL + NCX
DC = 16
L = 4
NIN = 15360
NE = 16
FF = 1024
CAPL = 256
CAPC = 32
NS = CAPL + CAPC
EPS = 1e-6
CH = 64
NCHUNK = T // CH
TBLK = [(0, 512), (512, 512), (1024, 512), (1536, 512), (2048, 256)]
ENGS = ("pe", "dve", "act", "pool", "sp")
NEG = -30000.0


class Buf:
    __slots__ = ("name", "w", "r")

    def __init__(self, name):
        self.name = name
        self.w = None
        self.r = {}


class Prog:
    def __init__(self, nc):
        self.nc = nc
        self.E = {"pe": nc.tensor, "dve": nc.vector, "act": nc.scalar, "pool": nc.gpsimd, "sp": nc.sync}
        self.sem = {}
        self.cnt = {}
        self.waited = {e: {} for e in ENGS}
        self.bufs = {}
        self.nops = 0
        for e in ENGS:
            self._mk(e)

    def _mk(self, k):
        if k not in self.sem:
            self.sem[k] = self.nc.alloc_semaphore(name="s_" + k)
            self.cnt[k] = 0

    def buf(self, x):
        if isinstance(x, Buf):
            return x
        n = x.tensor.name
        b = self.bufs.get(n)
        if b is None:
            b = self.bufs[n] = Buf(n)
        return b

    def _sync(self, eng, reads, writes, acc=False):
        need = {}
        for b in reads:
            if b.w is not None:
                k, v = b.w
                if need.get(k, 0) < v:
                    need[k] = v
        if not acc:
            for b in writes:
                if b.w is not None:
                    k, v = b.w
                    if need.get(k, 0) < v:
                        need[k] = v
                for k, v in b.r.items():
                    if need.get(k, 0) < v:
                        need[k] = v
        wd = self.waited[eng]
        e = self.E[eng]
        for k, v in need.items():
            if wd.get(k, 0) >= v:
                continue
            if k.startswith("d_"):
                v = self.cnt[k]
            wd[k] = v
            e.wait_ge(self.sem[k], v)

    def _commit(self, tok, reads, writes):
        k, v = tok
        for b in reads:
            if b.r.get(k, 0) < v:
                b.r[k] = v
        for b in writes:
            b.w = tok
            b.r = {}

    def op(self, eng, fn, r=(), w=(), acc=False):
        rb = [self.buf(x) for x in r]
        wb = [self.buf(x) for x in w]
        self._sync(eng, rb, wb, acc)
        ins = fn(self.E[eng])
        self.cnt[eng] += 1
        ins.then_inc(self.sem[eng], 1)
        self._commit((eng, self.cnt[eng]), rb, wb)
        self.nops += 1

    def dma(self, q, out, in_, key=None, r=None, w=None):
        rb = [self.buf(x) for x in (r if r is not None else [in_])]
        wb = [self.buf(x) for x in (w if w is not None else [out])]
        if key is None:
            key = q + ("_st" if "DRam" in type(out.tensor).__name__ else "_ld")
        k = "d_" + key
        self._mk(k)
        self._sync(q, rb, wb)
        ins = self.E[q].dma_start(out=out, in_=in_)
        self.cnt[k] += 16
        ins.then_inc(self.sem[k], 16)
        self._commit((k, self.cnt[k]), rb, wb)
        self.nops += 1

    def barrier(self):
        for e in ENGS:
            wd = self.waited[e]
            for k, v in self.cnt.items():
                if v > 0 and wd.get(k, 0) < v:
                    wd[k] = v
                    self.E[e].wait_ge(self.sem[k], v)

    def mm(self, out, lhsT, rhs, start, stop):
        self.op("pe", lambda e: e.matmul(out, lhsT, rhs, start=start, stop=stop),
                r=[lhsT, rhs], w=[out], acc=not start)

    def tr(self, out, in_, ident):
        self.op("pe", lambda e: e.transpose(out, in_, ident), r=[in_, ident], w=[out])

    def act(self, out, in_, func, scale=None, bias=None, eng="act"):
        r = [in_]
        kw = {}
        if scale is not None:
            kw["scale"] = scale
            if not isinstance(scale, (int, float)):
                r.append(scale)
        if bias is not None:
            kw["bias"] = bias
            if not isinstance(bias, (int, float)):
                r.append(bias)
        self.op("act", lambda e: e.activation(out=out, in_=in_, func=func, **kw), r=r, w=[out])

    def tt(self, eng, out, in0, in1, op):
        self.op(eng, lambda e: e.tensor_tensor(out=out, in0=in0, in1=in1, op=op), r=[in0, in1], w=[out])

    def ts(self, eng, out, in0, s1, s2, op0, op1=None):
        r = [in0]
        if not isinstance(s1, (int, float)):
            r.append(s1)
        if s2 is not None and not isinstance(s2, (int, float)):
            r.append(s2)
        if op1 is None:
            self.op(eng, lambda e: e.tensor_scalar(out=out, in0=in0, scalar1=s1, scalar2=None, op0=op0), r=r, w=[out])
        else:
            self.op(eng, lambda e: e.tensor_scalar(out=out, in0=in0, scalar1=s1, scalar2=s2, op0=op0, op1=op1), r=r, w=[out])

    def stt(self, out, in0, scalar, in1, op0, op1):
        r = [in0, in1]
        if not isinstance(scalar, (int, float)):
            r.append(scalar)
        self.op("dve", lambda e: e.scalar_tensor_tensor(out=out, in0=in0, scalar=scalar, in1=in1, op0=op0, op1=op1),
                r=r, w=[out])

    def copy(self, eng, out, in_):
        if eng == "act":
            self.op("act", lambda e: e.copy(out=out, in_=in_), r=[in_], w=[out])
        else:
            self.op(eng, lambda e: e.tensor_copy(out=out, in_=in_), r=[in_], w=[out])

    def memset(self, eng, out, val):
        self.op(eng, lambda e: e.memset(out, val), r=[], w=[out])


class Ring:
    def __init__(self, st, nc, name, n, shape, dtype, psum=False):
        self.t = []
        self.i = 0
        for k in range(n):
            alloc = nc.psum_tensor if psum else nc.sbuf_tensor
            self.t.append(st.enter_context(alloc(f"{name}{k}", list(shape), dtype)))

    def next(self):
        t = self.t[self.i % len(self.t)]
        self.i += 1
        return t


def _consts():
    c = {}
    c["ident"] = np.eye(128, dtype=np.float32)
    s = np.arange(CH)
    c["maskf"] = (s[:, None] <= s[None, :]).astype(np.int32)
    c["maskb"] = (s[:, None] >= s[None, :]).astype(np.int32)
    c["iotar"] = np.broadcast_to(np.arange(NS, dtype=np.float32)[None, :], (128, NS)).copy()
    p = np.arange(128, dtype=np.float32)
    c["iotac"] = np.stack([p, p + 128, p + 256], axis=1).copy()
    pos = np.arange(NL)
    row = (pos // 64).astype(np.float32)
    col = (pos % 64).astype(np.float32)
    half = 64
    inv_freq = (10000.0 ** (-np.arange(0, half, 2, dtype=np.float32) / half)).astype(np.float32)
    cosT = np.ones((128, T), np.float32)
    sinT = np.zeros((128, T), np.float32)
    ar = row[None, :] * inv_freq[:, None]
    ac = col[None, :] * inv_freq[:, None]
    cosT[0:32, :NL] = np.cos(ar); cosT[32:64, :NL] = np.cos(ar)
    cosT[64:96, :NL] = np.cos(ac); cosT[96:128, :NL] = np.cos(ac)
    sinT[0:32, :NL] = np.sin(ar); sinT[32:64, :NL] = np.sin(ar)
    sinT[64:96, :NL] = np.sin(ac); sinT[96:128, :NL] = np.sin(ac)
    c["cosT"] = cosT
    c["sinT"] = sinT
    R = np.zeros((128, 128), np.float32)
    for i in range(32):
        R[i, 32 + i] = -1.0
        R[32 + i, i] = 1.0
        R[64 + i, 96 + i] = -1.0
        R[96 + i, 64 + i] = 1.0
    c["RT"] = R.T.copy()
    inv = np.zeros((4, T), np.float32)
    for gi, w in enumerate((2, 4, 8, 16)):
        for (o, n) in ((0, NL), (NL, NCX)):
            ps = np.arange(n)
            lo = np.clip(ps - w // 2, 0, n - 1)
            hi = np.clip(ps + w // 2 - 1, 0, n - 1)
            inv[gi, o:o + n] = 1.0 / (hi - lo + 1)
    c["invcnt"] = np.broadcast_to(inv[:, None, :], (4, 128, T)).copy()
    c["attmask"], _, _ = _att_tables()
    sel = np.zeros((16, 16, 128), np.float32)
    for e in range(16):
        sel[e, e, :] = 1.0
    c["sel"] = sel
    return c


def _att_tiles():
    tiles = []
    for j in range(6):
        tiles.append(("I", j))
    for j in range(4):
        tiles.append(("A", j))
    for j in range(4):
        tiles.append(("Z", j))
    return tiles


def _att_tables():
    tiles = _att_tiles()
    mask = np.zeros((128, 14, 256), np.float32)
    dri = np.zeros((14, 128, 256), np.int64)
    dci = np.zeros((14, 128, 256), np.int64)
    kcol = np.arange(64)
    qcol = np.arange(64)
    cs = np.clip(qcol - 8, 0, 48)
    inwin = (kcol[:, None] >= cs[None, :]) & (kcol[:, None] < cs[None, :] + 16)
    dc = np.clip(kcol[:, None] - qcol[None, :] + 15, 0, 30)
    for ti, (kind, j) in enumerate(tiles):
        for a in range(2):
            for b in range(4):
                if kind == "I":
                    krp = 2 * j + a
                    valid = (b <= krp) and (krp < b + 8)
                    dr = krp - b - 4
                elif kind == "A":
                    valid = True
                    dr = 2 * j + a - b
                else:
                    valid = True
                    dr = 2 * j + a - b - 4
                drc = int(np.clip(dr + 7, 0, 14))
                blk = np.where(inwin, 0.0, NEG) if valid else np.full((64, 64), NEG)
                mask[a * 64:(a + 1) * 64, ti, b * 64:(b + 1) * 64] = blk
                dri[ti, a * 64:(a + 1) * 64, b * 64:(b + 1) * 64] = drc
                dci[ti, a * 64:(a + 1) * 64, b * 64:(b + 1) * 64] = dc
    return mask, dri, dci


def build(stop_after=99, dbg=(), nlayers=L):
    nc = bass.Bass("TRN2", target_bir_lowering=False)
    st = ExitStack()
    P = Prog(nc)

    class _NCW:
        n = 0

        def sbuf_tensor(self, name, shape, dt):
            _NCW.n += 1
            return nc.sbuf_tensor(f"{name}_u{_NCW.n}", shape, dt)

        def psum_tensor(self, name, shape, dt):
            _NCW.n += 1
            return nc.psum_tensor(f"{name}_u{_NCW.n}", shape, dt)
    ncw = _NCW()
    outs = {}

    def din(name, shape, dt=F32):
        return nc.dram_tensor(name, list(shape), dt, kind="ExternalInput").ap()

    def dscr(name, shape, dt):
        kind = "ExternalOutput" if name in dbg else "Internal"
        return nc.dram_tensor(name, list(shape), dt, kind=kind).ap()

    x_in = din("x", [NL, D])
    ctx_in = din("ctx", [NCX, D])
    cc_in = din("cc", [32, 128])
    w_mod = din("w_mod", [nlayers, D, 6 * D])
    vecs_in = din("vecs", [L, 144, 128])
    lbp_in = din("lbp", [64, 128])
    gfin_in = din("gfin", [16, 128])
    w_in = din("w_in", [nlayers, D, NIN])
    w_pool = din("w_pool", [nlayers, 4, 256, 256])
    rpbg = din("rpbg", [nlayers, 8, 128, 14, 256])
    w_branch = din("w_branch", [nlayers, 3, 1024, D])
    w_out = din("w_out", [nlayers, D, D])
    w_router = din("w_router", [nlayers, D, NE])
    w_gate = din("w_gate_e", [nlayers, NE, D, FF])
    w_up = din("w_up_e", [nlayers, NE, D, FF])
    w_down = din("w_down_e", [nlayers, NE, FF, D])
    cst = _consts()
    cin = {k: din("k_" + k, v.shape, I32 if v.dtype == np.int32 else F32) for k, v in cst.items()}
    y_out = nc.dram_tensor("y", [NL, D], F32, kind="ExternalOutput").ap()

    XT = [dscr(f"XT{c}", [128, T], F32) for c in range(DC)]
    QA = [dscr(f"QA{h}", [128, T], BF16) for h in range(8)]
    KF = [dscr(f"KF{h}", [128, T], BF16) for h in range(8)]
    KB = [dscr(f"KB{h}", [128, T], BF16) for h in range(8)]
    LFF = [dscr(f"LFF{h}", [128, T], F32) for h in range(8)]
    LFB = [dscr(f"LFB{h}", [128, T], F32) for h in range(8)]
    GA = [dscr(f"GA{h}", [128, T], BF16) for h in range(8)]
    UB = [dscr(f"UB{h}", [128, T], F32) for h in range(8)]
    QC = [dscr(f"QC{h}", [128, T], BF16) for h in range(8)]
    KC = [dscr(f"KC{h}", [128, T], BF16) for h in range(8)]
    VT = dscr("VT", [T, 1024], BF16)
    VC = dscr("VC", [T, 1024], BF16)
    UG = [dscr(f"UG{j}", [128, T], BF16) for j in range(48)]
    YA = [dscr(f"YA{h}", [128, T], BF16) for h in range(8)]
    YB = [dscr(f"YB{h}", [128, T], BF16) for h in range(8)]
    YC = [dscr(f"YC{h}", [128, T], BF16) for h in range(8)]
    MG = [dscr(f"MG{c}", [128, T], BF16) for c in range(DC)]
    YM = [dscr(f"YM{c}", [128, NE * 3 * 128], BF16) for c in range(DC)]

    def sb(name, shape, dt=F32):
        return st.enter_context(ncw.sbuf_tensor(name, list(shape), dt))

    ident = sb("ident", [128, 128])
    identb = sb("identb", [128, 128], BF16)
    onesb = sb("onesb", [128, 128], BF16)
    modT = sb("modT", [128, L, 96, 2])
    vecT = sb("vecT", [128, L, 144])
    lbT = sb("lbT", [128, 2, L, 8])
    oml = sb("oml", [128, 2, L, 8])
    gfT = sb("gfT", [128, 16])
    A1 = sb("A1", [128, L, 16, 2]); A2 = sb("A2", [128, L, 16, 2])
    pall = Ring(st, ncw, "pp", 8, [128, 512], F32, psum=True)
    pps = Ring.__new__(Ring); pps.t = pall.t[0:4]; pps.i = 0
    pacc = Ring.__new__(Ring); pacc.t = pall.t[4:8]; pacc.i = 0

    P.dma("sp", ident[:], cin["ident"][:, :])
    P.copy("dve", identb[:], ident[:])
    P.memset("dve", onesb[:], 1.0)

    with ExitStack() as ph:
        def psb(name, shape, dt=F32):
            return ph.enter_context(ncw.sbuf_tensor(name, list(shape), dt))
        xin = Ring(ph, ncw, "xin", 2, [128, D], F32)
        xst = Ring(ph, ncw, "xst", 2, [128, DC, 128], F32)
        for tt_ in range(T // 128):
            xt = xin.next()
            src = x_in[tt_ * 128:(tt_ + 1) * 128, :] if tt_ < 16 else ctx_in[(tt_ - 16) * 128:(tt_ - 15) * 128, :]
            P.dma("sp", xt[:], src)
            stg = xst.next()
            for g in range(4):
                ps = pps.next()
                for j in range(4):
                    c = g * 4 + j
                    P.tr(ps[:, j * 128:(j + 1) * 128], xt[:, c * 128:(c + 1) * 128], ident[:])
                P.copy("dve" if g % 2 == 0 else "act", stg[:, g * 4:(g + 1) * 4, :],
                       ps[:].rearrange("p (a b) -> p a b", a=4))
            for c in range(DC):
                P.dma("sp", XT[c][:, tt_ * 128:(tt_ + 1) * 128], stg[:, c, :])
        vtmp = psb("vtmp", [128, 128])
        for l in range(L):
            for (r0, nr) in ((0, 96), (96, 48)):
                P.dma("sp", vtmp[0:nr, :], vecs_in[l, r0:r0 + nr, :])
                ps = pps.next()
                P.tr(ps[:, 0:nr], vtmp[0:nr, :], ident[0:nr, 0:nr])
                P.copy("dve", vecT[:, l, r0:r0 + nr], ps[:, 0:nr])
        P.dma("sp", vtmp[0:64, :], lbp_in[:, :])
        ps = pps.next()
        P.tr(ps[:, 0:64], vtmp[0:64, :], ident[0:64, 0:64])
        lbe = psb("lbe", [128, 2, L, 8])
        P.act(lbe[:].rearrange("p a b c -> p (a b c)"), ps[:, 0:64], AF.Exp)
        lsum = psb("lsum", [128, 2, 8])
        P.tt("dve", lsum[:], lbe[:, :, 0, :], lbe[:, :, 1, :], ALU.add)
        P.tt("dve", lsum[:], lsum[:], lbe[:, :, 2, :], ALU.add)
        P.tt("dve", lsum[:], lsum[:], lbe[:, :, 3, :], ALU.add)
        lrec = psb("lrec", [128, 2, 8])
        P.op("dve", lambda e: e.reciprocal(out=lrec[:], in_=lsum[:]), r=[lsum[:]], w=[lrec[:]])
        P.memset("dve", lbT[:, :, 0, :], 0.0)
        ltmp = psb("ltmp", [128, 2, 8])
        for l in range(1, L):
            P.tt("dve", ltmp[:], lbe[:, :, l, :], lrec[:], ALU.mult)
            P.tt("dve", lbT[:, :, l, :], lbT[:, :, l - 1, :], ltmp[:], ALU.add)
        P.ts("dve", oml[:].rearrange("p a b c -> p (a b c)"), lbT[:].rearrange("p a b c -> p (a b c)"),
             -1.0, 1.0, ALU.mult, ALU.add)
        P.dma("sp", vtmp[0:16, :], gfin_in[:, :])
        ps = pps.next()
        P.tr(ps[:, 0:16], vtmp[0:16, :], ident[0:16, 0:16])
        P.copy("dve", gfT[:], ps[:, 0:16])
        P.dma("sp", vtmp[0:32, :], cc_in[:, :])
        scs = psb("scs", [32, 128])
        P.act(scs[:], vtmp[0:32, :], AF.Silu)
        ps = pps.next()
        P.tr(ps[:, 0:32], scs[:], ident[0:32, 0:32])
        scT = psb("scT", [128, 2, 16])
        P.copy("dve", scT[:].rearrange("p a b -> p (a b)"), ps[:, 0:32])
        wm = Ring(ph, ncw, "wm", 3, [128, DC, 512], F32)
        for l in range(nlayers):
            for cb in range(24):
                wt = wm.next()
                P.dma("sp", wt[:], w_mod[l, :, cb * 512:(cb + 1) * 512].rearrange("(c p) n -> p c n", p=128), key="w")
                ps = pps.next()
                for j in range(4):
                    for c in range(DC):
                        P.mm(ps[:, 2 * j:2 * j + 2], wt[:, c, j * 128:(j + 1) * 128], scT[:, :, c],
                             start=(c == 0), stop=(c == DC - 1))
                for j in range(4):
                    ft = cb * 4 + j
                    P.ts("dve", modT[:, l, ft, :], ps[:, 2 * j:2 * j + 2], vecT[:, l, ft:ft + 1], None, ALU.add)
            for (A, gi0, si) in ((A1, 96, 1), (A2, 112, 4)):
                for r in range(2):
                    P.stt(A[:, l, :, r], modT[:, l, si * 16:(si + 1) * 16, r], 1.0, vecT[:, l, gi0:gi0 + 16],
                          ALU.add, ALU.mult)
    P.barrier()
    if "modT" in dbg:
        o = nc.dram_tensor("o_modT", [128, L * 96 * 2], F32, kind="ExternalOutput").ap()
        P.dma("sp", o[:, :], modT[:].rearrange("p a b c -> p (a b c)"))
        o2 = nc.dram_tensor("o_lbT", [128, 2 * L * 8], F32, kind="ExternalOutput").ap()
        P.dma("sp", o2[:, :], lbT[:].rearrange("p a b c -> p (a b c)"))

    def norm_phase(ph, l, A, shift_i, hT, tile_cb=None):
        xr = Ring(ph, ncw, "nx", 2, [128, DC, 256], F32)
        sqr = Ring(ph, ncw, "nsq", 2, [128, 256], BF16)
        rsr = Ring(ph, ncw, "nrs", 2, [128, 256], F32)
        tmr = Ring(ph, ncw, "ntm", 3, [128, 256], F32)
        for t0 in range(0, T, 256):
            r = 0 if t0 < NL else 1
            xt = xr.next()
            for c in range(DC):
                P.dma("sp", xt[:, c, :], XT[c][:, t0:t0 + 256])
            ps = pps.next()
            for c in range(DC):
                sq = sqr.next()
                P.act(sq[:], xt[:, c, :], AF.Square)
                P.mm(ps[:, 0:256], onesb[:], sq[:], start=(c == 0), stop=(c == DC - 1))
            rs = rsr.next()
            P.ts("dve", rs[:], ps[:, 0:256], 1.0 / D, EPS, ALU.mult, ALU.add)
            P.act(rs[:], rs[:], AF.Sqrt)
            P.op("dve", lambda e: e.reciprocal(out=rs[:], in_=rs[:]), r=[rs[:]], w=[rs[:]])
            for c in range(DC):
                tm = tmr.next()
                P.tt("dve", tm[:], xt[:, c, :], rs[:], ALU.mult)
                if tile_cb is None:
                    P.ts("dve", hT[:, c, t0:t0 + 256], tm[:], A[:, l, c, r:r + 1],
                         modT[:, l, shift_i * 16 + c, r:r + 1], ALU.mult, ALU.add)
                else:
                    tile_cb(t0, c, r, tm)

    if stop_after < 1:
        nlayers = 0

    for l in range(nlayers):
        with ExitStack() as ph:
            hT = ph.enter_context(ncw.sbuf_tensor("hT", [128, DC, T], BF16))
            with ExitStack() as ph2:
                norm_phase(ph2, l, A1, 0, hT)
            P.barrier()
            if l == 0 and "hT" in dbg:
                o = nc.dram_tensor("o_hT", [128, DC * T], BF16, kind="ExternalOutput").ap()
                P.dma("sp", o[:, :], hT[:].rearrange("p a b -> p (a b)"))
            wr = Ring(ph, ncw, "wi", 3, [128, DC, 512], BF16)
            cosT = ph.enter_context(ncw.sbuf_tensor("cosT", [128, T], F32))
            sinT = ph.enter_context(ncw.sbuf_tensor("sinT", [128, T], F32))
            RTb = ph.enter_context(ncw.sbuf_tensor("RTb", [128, 128], BF16))
            P.dma("sp", cosT[:], cin["cosT"][:, :])
            P.dma("sp", sinT[:], cin["sinT"][:, :])
            P.dma("pool", RTb[:], cin["RT"][:, :])
            ev32 = Ring(ph, ncw, "ev32", 3, [128, 512], F32)
            ev16 = Ring(ph, ncw, "ev16", 3, [128, 512], BF16)
            evb = Ring(ph, ncw, "evb", 3, [128, 512], F32)
            for cb in range(NIN // 512):
                wt = wr.next()
                P.dma("pool", wt[:], w_in[l, :, cb * 512:(cb + 1) * 512].rearrange("(c p) n -> p c n", p=128), key="w")
                split = cb // 2 if cb < 18 else 9
                if split in (3, 8):
                    dst = VT if split == 3 else VC
                    c0 = (cb % 2) * 512
                    for tt_ in range(T // 128):
                        ps = pps.next()
                        for c in range(DC):
                            P.mm(ps[:], hT[:, c, tt_ * 128:(tt_ + 1) * 128], wt[:, c, :],
                                 start=(c == 0), stop=(c == DC - 1))
                        o = ev16.next()
                        P.copy("act" if tt_ % 2 else "dve", o[:], ps[:])
                        P.dma("sp", dst[tt_ * 128:(tt_ + 1) * 128, c0:c0 + 512], o[:])
                    continue
                for j in range(4):
                    ft = cb * 4 + j
                    hh = ft % 8
                    for (t0, tn) in TBLK:
                        ps = pps.next()
                        for c in range(DC):
                            P.mm(ps[:, 0:tn], wt[:, c, j * 128:(j + 1) * 128], hT[:, c, t0:t0 + tn],
                                 start=(c == 0), stop=(c == DC - 1))
                        if split in (0, 4):
                            o = ev16.next()
                            P.act(o[:, 0:tn], ps[:, 0:tn], AF.Silu)
                            P.dma("sp", (QA if split == 0 else GA)[hh][:, t0:t0 + tn], o[:, 0:tn])
                        elif split in (1, 2):
                            dr = split - 1
                            s = ev32.next()
                            P.act(s[:, 0:tn], ps[:, 0:tn], AF.Sigmoid)
                            f = evb.next()
                            P.ts("dve", f[:, 0:tn], s[:, 0:tn], oml[:, dr, l, hh:hh + 1], lbT[:, dr, l, hh:hh + 1],
                                 ALU.mult, ALU.add)
                            k = ev16.next()
                            P.ts("dve", k[:, 0:tn], f[:, 0:tn], -1.0, 1.0, ALU.mult, ALU.add)
                            P.dma("sp", (KF if dr == 0 else KB)[hh][:, t0:t0 + tn], k[:, 0:tn])
                            lf = ev32.next()
                            P.act(lf[:, 0:tn], f[:, 0:tn], AF.Ln)
                            P.dma("sp", (LFF if dr == 0 else LFB)[hh][:, t0:t0 + tn], lf[:, 0:tn])
                        elif split == 5:
                            o = ev32.next()
                            P.copy("act", o[:, 0:tn], ps[:, 0:tn])
                            P.dma("sp", UB[hh][:, t0:t0 + tn], o[:, 0:tn])
                        elif split in (6, 7):
                            ub = ev16.next()
                            P.copy("act", ub[:, 0:tn], ps[:, 0:tn])
                            ps2 = pps.next()
                            P.mm(ps2[:, 0:tn], RTb[:], ub[:, 0:tn], start=True, stop=True)
                            t1 = ev32.next()
                            P.tt("dve", t1[:, 0:tn], ub[:, 0:tn], cosT[:, t0:t0 + tn], ALU.mult)
                            t2 = evb.next()
                            P.tt("dve", t2[:, 0:tn], ps2[:, 0:tn], sinT[:, t0:t0 + tn], ALU.mult)
                            o = ev16.next()
                            P.tt("dve", o[:, 0:tn], t1[:, 0:tn], t2[:, 0:tn], ALU.add)
                            P.dma("sp", (QC if split == 6 else KC)[hh][:, t0:t0 + tn], o[:, 0:tn])
                        else:
                            o = ev16.next()
                            P.act(o[:, 0:tn], ps[:, 0:tn], AF.Sigmoid)
                            P.dma("sp", UG[ft - 72][:, t0:t0 + tn], o[:, 0:tn])
        P.barrier()
        if stop_after < 2:
            break

        with ExitStack() as ph:
            def hs(name, shape, dt=F32):
                return ph.enter_context(ncw.sbuf_tensor(name, list(shape), dt))
            maskf = hs("maskf", [CH, CH], I32); maskb = hs("maskb", [CH, CH], I32)
            P.dma("sp", maskf[:], cin["maskf"][:, :]); P.dma("sp", maskb[:], cin["maskb"][:, :])
            onec = hs("onec", [128, 1])
            P.memset("dve", onec[:], 1.0)
            q = hs("hq", [128, T], BF16); kf = hs("hkf", [128, T], BF16); kb = hs("hkb", [128, T], BF16)
            ga = hs("hga", [128, T], BF16)
            lff = hs("hlff", [128, T]); lfb = hs("hlfb", [128, T])
            Bf = hs("hBf", [128, T]); Eb = hs("hEb", [128, T]); Br = hs("hBr", [128, T]); Et = hs("hEt", [128, T])
            qdf = hs("hqdf", [128, T], BF16); kdf = hs("hkdf", [128, T], BF16)
            qdb = hs("hqdb", [128, T], BF16); kdb = hs("hkdb", [128, T], BF16)
            vt = hs("hv", [CH, NCHUNK, 128], BF16)
            oTf = hs("hoTf", [128, T]); oTb = hs("hoTb", [128, T])
            sqo = hs("hsq", [128, T], BF16)
            DF = hs("hDF", [128, NCHUNK]); DB = hs("hDB", [128, NCHUNK]); dtmp = hs("hdtmp", [128, NCHUNK])
            Sf = hs("hSf", [128, 128]); Sb = hs("hSb", [128, 128])
            Sfb = Ring(ph, ncw, "hSfb", 2, [128, 128], BF16); Sbb = Ring(ph, ncw, "hSbb", 2, [128, 128], BF16)
            Atf = Ring(ph, ncw, "hAf", 4, [CH, CH], BF16); Atb = Ring(ph, ncw, "hAb", 4, [CH, CH], BF16)
            for rr in (Atf, Atb):
                for t_ in rr.t:
                    P.memset("dve", t_[:], 0.0)
            kdt = Ring(ph, ncw, "hkdt", 8, [CH, 128], BF16)
            stmp = Ring(ph, ncw, "hstmp", 4, [128, 128], F32)
            yr = Ring(ph, ncw, "hy", 2, [128, 512], F32); yo = Ring(ph, ncw, "hyo", 2, [128, 512], BF16)
            rsr = Ring(ph, ncw, "hrs", 2, [128, 512], F32)

            def v3(ap):
                return ap.rearrange("p (n s) -> p n s", s=CH)

            forder = [32, 33, 34, 35] + list(range(32))
            border = list(range(35, -1, -1))
            for h in range(8):
                P.dma("sp", q[:], QA[h][:, :]); P.dma("sp", kf[:], KF[h][:, :]); P.dma("sp", kb[:], KB[h][:, :])
                P.dma("sp", lff[:], LFF[h][:, :]); P.dma("sp", lfb[:], LFB[h][:, :]); P.dma("sp", ga[:], GA[h][:, :])
                P.dma("sp", vt[:], VT[:, h * 128:(h + 1) * 128].rearrange("(n s) v -> s n v", s=CH))
                P.op("dve", lambda e: e.tensor_tensor_scan(out=Bf[:, NL:T], data0=onec[:, 0:1].to_broadcast([128, NCX]),
                                                           data1=lff[:, NL:T], initial=0.0, op0=ALU.mult, op1=ALU.add),
                     r=[lff[:], onec[:]], w=[Bf[:]])
                P.op("dve", lambda e: e.tensor_tensor_scan(out=Bf[:, 0:NL], data0=onec[:, 0:1].to_broadcast([128, NL]),
                                                           data1=lff[:, 0:NL], initial=Bf[:, T - 1:T], op0=ALU.mult, op1=ALU.add),
                     r=[lff[:], onec[:], Bf[:]], w=[Bf[:]])
                P.op("dve", lambda e: e.tensor_tensor_scan(out=Eb[:, :], data0=onec[:, 0:1].to_broadcast([128, T]),
                                                           data1=lfb[:, :], initial=0.0, op0=ALU.mult, op1=ALU.add),
                     r=[lfb[:], onec[:]], w=[Eb[:]])
                P.tt("dve", Eb[:], Eb[:], lfb[:], ALU.subtract)
                Bfm = v3(Bf[:])[:, :, CH // 2]
                Ebm = v3(Eb[:])[:, :, CH // 2]
                P.tt("dve", dtmp[:, 0:NCHUNK - 1], Bfm[:, 1:NCHUNK], Bfm[:, 0:NCHUNK - 1], ALU.subtract)
                P.tt("dve", dtmp[:, NCHUNK - 1:NCHUNK], Bfm[:, 0:1], Bfm[:, NCHUNK - 1:NCHUNK], ALU.subtract)
                P.act(DF[:], dtmp[:], AF.Exp)
                P.tt("dve", dtmp[:, 1:NCHUNK], Ebm[:, 1:NCHUNK], Ebm[:, 0:NCHUNK - 1], ALU.subtract)
                P.act(DB[:, 1:NCHUNK], dtmp[:, 1:NCHUNK], AF.Exp)
                P.tt("dve", v3(Br[:]), v3(Bf[:]), Bfm.unsqueeze(2).to_broadcast([128, NCHUNK, CH]), ALU.subtract)
                P.act(Et[:], Br[:], AF.Exp)
                P.tt("dve", qdf[:], q[:], Et[:], ALU.mult)
                P.act(Et[:], Br[:], AF.Exp, scale=-1.0)
                P.tt("dve", kdf[:], kf[:], Et[:], ALU.mult)
                P.tt("dve", v3(Br[:]), v3(Eb[:]), Ebm.unsqueeze(2).to_broadcast([128, NCHUNK, CH]), ALU.subtract)
                P.act(Et[:], Br[:], AF.Exp)
                P.tt("dve", kdb[:], kb[:], Et[:], ALU.mult)
                P.act(Et[:], Br[:], AF.Exp, scale=-1.0)
                P.tt("dve", qdb[:], q[:], Et[:], ALU.mult)
                P.memset("dve", Sf[:], 0.0); P.memset("dve", Sb[:], 0.0)
                sfb = Sfb.next(); sbb = Sbb.next()
                P.memset("dve", sfb[:], 0.0); P.memset("dve", sbb[:], 0.0)
                cur = {"f": sfb, "b": sbb}
                def cfg(dr_):
                    return ((qdf, kdf, maskf, Sf, Sfb, Atf, oTf) if dr_ == "f"
                            else (qdb, kdb, maskb, Sb, Sbb, Atb, oTb))

                def h_stageA(i):
                    res = {}
                    for dr_ in ("f", "b"):
                        n = forder[i] if dr_ == "f" else border[i]
                        qd, kd, msk, S, Sring, Ar, oT = cfg(dr_)
                        cs = slice(n * CH, (n + 1) * CH)
                        ps_s = pps.next()
                        P.mm(ps_s[0:CH, 0:CH], kd[:, cs], qd[:, cs], True, True)
                        A = Ar.next()
                        P.op("dve", lambda e: e.copy_predicated(out=A[:], mask=msk[:], data=ps_s[0:CH, 0:CH]),
                             r=[msk[:], ps_s[:]], w=[A[:]])
                        ps_k = pps.next()
                        pkb = ps_k[:].bitcast(BF16)
                        P.tr(pkb[0:CH, 0:128], kd[:, cs], identb[:])
                        kt = kdt.next()
                        P.copy("act", kt[:], pkb[0:CH, 0:128])
                        res[dr_] = (A, kt)
                    return res

                nxt = h_stageA(0)
                for i in range(NCHUNK):
                    curA = nxt
                    if i + 1 < NCHUNK:
                        nxt = h_stageA(i + 1)
                    for dr_ in ("f", "b"):
                        n = forder[i] if dr_ == "f" else border[i]
                        qd, kd, msk, S, Sring, Ar, oT = cfg(dr_)
                        cs = slice(n * CH, (n + 1) * CH)
                        A, kt = curA[dr_]
                        ps_o = pacc.next()
                        P.mm(ps_o[:, 0:CH], vt[:, n, :], A[:], True, False)
                        P.mm(ps_o[:, 0:CH], cur[dr_][:], qd[:, cs], False, True)
                        P.copy("act", oT[:, cs], ps_o[:, 0:CH])
                        if i < NCHUNK - 1:
                            ps_p = pacc.next()
                            P.mm(ps_p[:, 0:128], kt[:], vt[:, n, :], True, True)
                            tmp = stmp.next()
                            P.tt("dve", tmp[:], S[:], ps_p[:, 0:128], ALU.add)
                            dcol = DF[:, n:n + 1] if dr_ == "f" else DB[:, n:n + 1]
                            P.ts("dve", S[:], tmp[:], dcol, None, ALU.mult)
                            nb = Sring.next()
                            P.act(nb[:], tmp[:], AF.Copy, scale=dcol)
                            cur[dr_] = nb
                P.tt("dve", oTf[:], oTf[:], oTb[:], ALU.add)
                P.act(sqo[:], oTf[:], AF.Square)
                for (t0, tn) in TBLK:
                    ps = pps.next()
                    P.mm(ps[:, 0:tn], onesb[:], sqo[:, t0:t0 + tn], True, True)
                    rs = rsr.next()
                    P.ts("dve", rs[:, 0:tn], ps[:, 0:tn], 1.0 / 128, EPS, ALU.mult, ALU.add)
                    P.act(rs[:, 0:tn], rs[:, 0:tn], AF.Sqrt)
                    P.op("dve", lambda e: e.reciprocal(out=rs[:, 0:tn], in_=rs[:, 0:tn]), r=[rs[:]], w=[rs[:]])
                    y1 = yr.next()
                    P.tt("dve", y1[:, 0:tn], oTf[:, t0:t0 + tn], rs[:, 0:tn], ALU.mult)
                    y2 = yo.next()
                    P.stt(y2[:, 0:tn], y1[:, 0:tn], vecT[:, l, 128 + h:129 + h], ga[:, t0:t0 + tn], ALU.mult, ALU.mult)
                    P.dma("sp", YA[h][:, t0:t0 + tn], y2[:, 0:tn])
        P.barrier()
        if stop_after < 3:
            break
        with ExitStack() as ph:
            def hs(name, shape, dt=F32):
                return ph.enter_context(ncw.sbuf_tensor(name, list(shape), dt))
            wp = hs("wp", [128, 4, 2, 256], BF16)
            P.dma("pool", wp[:], w_pool[l].rearrange("g (c p) e -> p g c e", p=128), key="w")
            PADT = T + 64
            ua = Ring(ph, ncw, "pu", 2, [128, PADT], F32)
            sa = hs("psa", [128, PADT]); sbb_ = hs("psb", [128, PADT])
            inv = hs("pinv", [128, T])
            dT = hs("pdT", [128, 2, T], BF16)
            dtm = hs("pdtm", [128, T])
            yo = Ring(ph, ncw, "pyo", 2, [128, 512], BF16)
            segs = ((16, 0, NL), (NL + 48, NL, NCX))
            for gi, w_ in enumerate((2, 4, 8, 16)):
                P.dma("sp", inv[:], cin["invcnt"][gi])
                for cc in range(2):
                    hh = gi * 2 + cc
                    u = ua.next()
                    P.memset("dve", u[:], 0.0)
                    for (o, t0, n) in segs:
                        P.dma("sp", u[:, o:o + n], UB[hh][:, t0:t0 + n])
                    for (o, t0, n) in segs:
                        lo, hi = o - 8, o + n + 8
                        P.tt("dve", sa[:, lo:hi], u[:, lo - 1:hi - 1], u[:, lo:hi], ALU.add)
                        src = sa
                        if w_ >= 4:
                            P.tt("dve", sbb_[:, lo + 2:hi - 2], sa[:, lo + 1:hi - 3], sa[:, lo + 3:hi - 1], ALU.add)
                            src = sbb_
                        if w_ >= 8:
                            P.tt("dve", sa[:, lo + 4:hi - 4], sbb_[:, lo + 2:hi - 6], sbb_[:, lo + 6:hi - 2], ALU.add)
                            src = sa
                        if w_ >= 16:
                            P.tt("dve", sbb_[:, o:o + n], sa[:, o - 4:o + n - 4], sa[:, o + 4:o + n + 4], ALU.add)
                            src = sbb_
                        P.tt("dve", dtm[:, t0:t0 + n], src[:, o:o + n], inv[:, t0:t0 + n], ALU.mult)
                        P.tt("dve", dT[:, cc, t0:t0 + n], dtm[:, t0:t0 + n], u[:, o:o + n], ALU.subtract)
                for et in range(2):
                    hh = gi * 2 + et
                    for (t0, tn) in TBLK:
                        ps = pps.next()
                        for cc in range(2):
                            P.mm(ps[:, 0:tn], wp[:, gi, cc, et * 128:(et + 1) * 128], dT[:, cc, t0:t0 + tn],
                                 cc == 0, cc == 1)
                        o_ = yo.next()
                        P.act(o_[:, 0:tn], ps[:, 0:tn], AF.Copy, scale=vecT[:, l, 136 + hh:137 + hh])
                        P.dma("sp", YB[hh][:, t0:t0 + tn], o_[:, 0:tn])
        P.barrier()
        if stop_after < 4:
            break
        with ExitStack() as ph:
            def hs(name, shape, dt=F32):
                return ph.enter_context(ncw.sbuf_tensor(name, list(shape), dt))
            am = hs("am", [128, 14, 256])
            P.dma("sp", am[:], cin["attmask"][:, :, :])
            qT = hs("aq", [128, T], BF16); kT = hs("ak", [128, T], BF16)
            vv = hs("av", [128, T // 128, 128], BF16)
            BT = hs("aBT", [128, 14, 256])
            sc_ = Ring(ph, ncw, "asc", 4, [128, 256], F32)
            pTr = Ring(ph, ncw, "apT", 8, [128, 256], BF16)
            rec = Ring(ph, ncw, "arec", 2, [128, 256], F32)
            yo = Ring(ph, ncw, "ayo", 2, [128, 256], BF16)
            SCALE = 128.0 ** -0.5
            for h in range(8):
                P.dma("sp", qT[:], QC[h][:, :]); P.dma("sp", kT[:], KC[h][:, :])
                P.dma("sp", vv[:], VC[:, h * 128:(h + 1) * 128].rearrange("(n p) v -> p n v", p=128))
                P.dma("sp", BT[:], rpbg[l, h])
                P.tt("dve", BT[:], BT[:], am[:], ALU.add)
                for qb in range(9):
                    q0 = qb * 256
                    if qb == 0:
                        kts = [(128 * j, 6 + j) for j in range(4)]
                    elif qb == 7:
                        kts = [((24 + 2 * j) * 64, 10 + j) for j in range(4)]
                    elif qb == 8:
                        kts = []
                    else:
                        kts = [((4 * qb - 4 + 2 * j) * 64, j) for j in range(6)]
                    kts = kts + [(NL, None), (NL + 128, None)]
                    ps_o = pacc.next(); ps_d = pacc.next()

                    def a_stage(i):
                        ks, bi = kts[i]
                        ps_s = pps.next()
                        P.mm(ps_s[:, 0:256], kT[:, ks:ks + 128], qT[:, q0:q0 + 256], True, True)
                        pT = pTr.next()
                        if bi is not None:
                            t_ = sc_.next()
                            P.stt(t_[:], ps_s[:, 0:256], SCALE, BT[:, bi, :], ALU.mult, ALU.add)
                            P.act(pT[:], t_[:], AF.Exp)
                        else:
                            P.act(pT[:], ps_s[:, 0:256], AF.Exp, scale=SCALE)
                        return pT

                    LOOK = 3
                    pend = [a_stage(i) for i in range(min(LOOK, len(kts)))]
                    for i, (ks, bi) in enumerate(kts):
                        pT = pend.pop(0)
                        if i + LOOK < len(kts):
                            pend.append(a_stage(i + LOOK))
                        P.mm(ps_o[:, 0:256], vv[:, ks // 128, :], pT[:], i == 0, i == len(kts) - 1)
                        P.mm(ps_d[:, 0:256], onesb[:], pT[:], i == 0, i == len(kts) - 1)
                    rc = rec.next()
                    P.op("dve", lambda e: e.reciprocal(out=rc[:], in_=ps_d[:, 0:256]), r=[ps_d[:]], w=[rc[:]])
                    o_ = yo.next()
                    P.tt("dve", o_[:], ps_o[:, 0:256], rc[:], ALU.mult)
                    P.dma("sp", YC[h][:, q0:q0 + 256], o_[:])
        P.barrier()
        if stop_after < 5:
            break

        with ExitStack() as ph:
            wb = ph.enter_context(ncw.sbuf_tensor("wb", [128, 3, 8, D], BF16))
            for j in range(3):
                for hf in range(2):
                    P.dma("pool", wb[:, j, hf * 4:(hf + 1) * 4, :],
                          w_branch[l, j, hf * 512:(hf + 1) * 512, :].rearrange("(c p) d -> p c d", p=128), key="w")
            yr3 = [Ring(ph, ncw, f"my{j}", 2, [128, 8, 512], BF16) for j in range(3)]
            gr = Ring(ph, ncw, "mg_g", 4, [128, 512], BF16)
            ar = Ring(ph, ncw, "mg_a", 2, [128, 512], F32)
            t2r = Ring(ph, ncw, "mg_t", 2, [128, 512], F32)
            mo = Ring(ph, ncw, "mg_o", 2, [128, 512], BF16)
            gr8 = Ring(ph, ncw, "mg_g8", 8, [128, 512], BF16)
            its = [(bi, dt_) for bi in range(len(TBLK)) for dt_ in range(DC)]
            ystate = {}

            def m_loads(it):
                bi, dt_ = it
                t0, tn = TBLK[bi]
                if dt_ == 0:
                    ys = [rr.next() for rr in yr3]
                    for j, Y in enumerate((YA, YB, YC)):
                        for c in range(8):
                            P.dma("sp", ys[j][:, c, 0:tn], Y[c][:, t0:t0 + tn])
                    ystate[bi] = ys
                gs = []
                for j in range(3):
                    g = gr8.next()
                    P.dma("sp", g[:, 0:tn], UG[j * 16 + dt_][:, t0:t0 + tn])
                    gs.append(g)
                return gs

            pend = m_loads(its[0])
            for ii, (bi, dt_) in enumerate(its):
                t0, tn = TBLK[bi]
                gs = pend
                if ii + 1 < len(its):
                    pend = m_loads(its[ii + 1])
                ys = ystate[bi]
                acc = ar.next()
                for j in range(3):
                    g = gs[j]
                    ps = pall.next()
                    for c in range(8):
                        P.mm(ps[:, 0:tn], wb[:, j, c, dt_ * 128:(dt_ + 1) * 128], ys[j][:, c, 0:tn], c == 0, c == 7)
                    if j == 0:
                        P.tt("dve", acc[:, 0:tn], ps[:, 0:tn], g[:, 0:tn], ALU.mult)
                    else:
                        t2 = t2r.next()
                        P.tt("dve", t2[:, 0:tn], ps[:, 0:tn], g[:, 0:tn], ALU.mult)
                        if j == 1:
                            P.tt("pool", acc[:, 0:tn], acc[:, 0:tn], t2[:, 0:tn], ALU.add)
                        else:
                            o_ = mo.next()
                            P.tt("pool", o_[:, 0:tn], acc[:, 0:tn], t2[:, 0:tn], ALU.add)
                            P.dma("sp", MG[dt_][:, t0:t0 + tn], o_[:, 0:tn])
        P.barrier()
        with ExitStack() as ph:
            wo = ph.enter_context(ncw.sbuf_tensor("wo", [128, DC, D], BF16))
            for qd_ in range(4):
                P.dma("pool", wo[:, qd_ * 4:(qd_ + 1) * 4, :],
                      w_out[l, qd_ * 512:(qd_ + 1) * 512, :].rearrange("(c p) d -> p c d", p=128), key="w")
            mgr = Ring(ph, ncw, "wo_m", 2, [128, DC, 512], BF16)
            xr = Ring(ph, ncw, "wo_x", 3, [128, 512], F32)
            xo = Ring(ph, ncw, "wo_o", 3, [128, 512], F32)
            xr = Ring(ph, ncw, "wo_x4", 5, [128, 512], F32)
            its = [(bi, et) for bi in range(len(TBLK)) for et in range(DC)]
            mstate = {}

            def w_loads(it):
                bi, et = it
                t0, tn = TBLK[bi]
                if et == 0:
                    mg = mgr.next()
                    for c in range(DC):
                        P.dma("sp", mg[:, c, 0:tn], MG[c][:, t0:t0 + tn])
                    mstate[bi] = mg
                xt = xr.next()
                P.dma("sp", xt[:, 0:tn], XT[et][:, t0:t0 + tn])
                return xt

            pend = [w_loads(its[0]), w_loads(its[1])]
            for ii, (bi, et) in enumerate(its):
                t0, tn = TBLK[bi]
                r = 0 if t0 < NL else 1
                xt = pend.pop(0)
                if ii + 2 < len(its):
                    pend.append(w_loads(its[ii + 2]))
                mg = mstate[bi]
                ps = pall.next()
                for c in range(DC):
                    P.mm(ps[:, 0:tn], wo[:, c, et * 128:(et + 1) * 128], mg[:, c, 0:tn], c == 0, c == DC - 1)
                o_ = xo.next()
                P.stt(o_[:, 0:tn], ps[:, 0:tn], modT[:, l, 32 + et, r:r + 1], xt[:, 0:tn], ALU.mult, ALU.add)
                P.dma("sp", XT[et][:, t0:t0 + tn], o_[:, 0:tn])
        P.barrier()
        if stop_after < 6:
            break
        with ExitStack() as ph:
            def hs(name, shape, dt=F32):
                return ph.enter_context(ncw.sbuf_tensor(name, list(shape), dt))
            posm = hs("posm", [NE, T]); gw = hs("gwT", [NE, T])
            posmt = hs("posmt", [128, T // 128, NE])
            wrt = hs("wrt", [128, DC, NE])
            iotar = hs("iotar", [128, NS])
            P.dma("sp", iotar[:], cin["iotar"][:, :])
            P.dma("sp", wrt[:], w_router[l].rearrange("(c p) e -> p c e", p=128))
            m8 = hs("m8", [NE, 8]); thr = hs("thr", [NE, 2]); onee = hs("onee", [NE, 1])
            hk = ExitStack()
            h2tok = hk.enter_context(ncw.sbuf_tensor("h2tok", [128, T // 128, D], BF16))
            pk = ExitStack()
            affT = pk.enter_context(ncw.sbuf_tensor("affT", [NE, T], F32))
            wrk = pk.enter_context(ncw.sbuf_tensor("awrk", [NE, T], F32))
            mk = pk.enter_context(ncw.sbuf_tensor("mkT", [NE, T], F32))
            with ExitStack() as ph2:
                h2f = Ring(ph2, ncw, "h2f", 3, [128, 256], F32)
                h2b = Ring(ph2, ncw, "h2b", 3, [128, 256], BF16)
                sm = Ring(ph2, ncw, "rsm", 2, [128, 2, NE], F32)
                sm1 = Ring(ph2, ncw, "rsm1", 6, [128, 2], F32)
                state = {}

                def cb(t0, c, r, tm):
                    if c == 0:
                        state["ps"] = [pacc.next(), pacc.next()]
                    psr2 = state["ps"]
                    hf = h2f.next()
                    P.ts("dve", hf[:], tm[:], A2[:, l, c, r:r + 1], modT[:, l, 3 * 16 + c, r:r + 1], ALU.mult, ALU.add)
                    for s_ in range(2):
                        P.mm(psr2[s_][:, 0:NE], hf[:, s_ * 128:(s_ + 1) * 128], wrt[:, c, :],
                             c == 0, c == DC - 1)
                    hb = h2b.next()
                    P.copy("act", hb[:], hf[:])
                    pt = pps.next()
                    ptb = pt[:].bitcast(BF16)
                    for s_ in range(2):
                        P.tr(ptb[:, s_ * 128:(s_ + 1) * 128], hb[:, s_ * 128:(s_ + 1) * 128], identb[:])
                    tt0 = t0 // 128
                    P.copy("act", h2tok[:, tt0:tt0 + 2, c * 128:(c + 1) * 128],
                           ptb[:, 0:256].rearrange("p (s d) -> p s d", s=2))
                    if c == DC - 1:
                        mx = sm1.next()
                        for s_ in range(2):
                            P.op("dve", lambda e: e.tensor_reduce(out=mx[:, s_:s_ + 1], in_=psr2[s_][:, 0:NE],
                                                                  axis=mybir.AxisListType.X, op=ALU.max),
                                 r=[psr2[s_][:]], w=[mx[:]])
                        P.ts("dve", mx[:], mx[:], -1.0, None, ALU.mult)
                        ex = sm.next()
                        for s_ in range(2):
                            P.act(ex[:, s_, :], psr2[s_][:, 0:NE], AF.Exp, bias=mx[:, s_:s_ + 1])
                        su = sm1.next()
                        P.op("dve", lambda e: e.tensor_reduce(out=su[:], in_=ex[:], axis=mybir.AxisListType.X, op=ALU.add),
                             r=[ex[:]], w=[su[:]])
                        P.op("dve", lambda e: e.reciprocal(out=su[:], in_=su[:]), r=[su[:]], w=[su[:]])
                        for s_ in range(2):
                            P.ts("dve", ex[:, s_, :], ex[:, s_, :], su[:, s_:s_ + 1], None, ALU.mult)
                            pa = pps.next()
                            P.tr(pa[0:NE, 0:128], ex[:, s_, :], ident[:])
                            P.copy("act", affT[:, t0 + s_ * 128:t0 + (s_ + 1) * 128], pa[0:NE, 0:128])

                norm_phase(ph2, l, A2, 3, None, tile_cb=cb)
            P.barrier()
            P.memset("dve", onee[:], 1.0)
            P.copy("dve", wrk[:], affT[:])
            for si, (a, b, rounds) in enumerate(((0, NL, CAPL // 8), (NL, T, CAPC // 8))):
                for _ in range(rounds):
                    P.op("dve", lambda e: e.max(out=m8[:], in_=wrk[:, a:b]), r=[wrk[:]], w=[m8[:]])
                    P.op("dve", lambda e: e.match_replace(out=wrk[:, a:b], in_to_replace=m8[:], in_values=wrk[:, a:b],
                                                          imm_value=-1.0), r=[m8[:], wrk[:]], w=[wrk[:]])
                P.copy("dve", thr[:, si:si + 1], m8[:, 7:8])
                P.ts("dve", mk[:, a:b], affT[:, a:b], thr[:, si:si + 1], None, ALU.is_ge)
                P.op("dve", lambda e: e.tensor_tensor_scan(out=posm[:, a:b], data0=onee[:, 0:1].to_broadcast([NE, b - a]),
                                                           data1=mk[:, a:b], initial=float(0 if si == 0 else CAPL),
                                                           op0=ALU.mult, op1=ALU.add),
                     r=[mk[:], onee[:]], w=[posm[:]])
            P.tt("dve", gw[:], mk[:], affT[:], ALU.mult)
            P.tt("dve", posm[:], posm[:], mk[:], ALU.mult)
            P.ts("dve", posm[:], posm[:], -1.0, None, ALU.add)
            for tt_ in range(T // 128):
                pa = pps.next()
                P.tr(pa[:, 0:NE], posm[:, tt_ * 128:(tt_ + 1) * 128], ident[0:NE, 0:NE])
                P.copy("act", posmt[:, tt_, :], pa[:, 0:NE])
            if l == 0 and "affT" in dbg:
                o = nc.dram_tensor("o_affT", [NE, T], F32, kind="ExternalOutput").ap()
                P.dma("sp", o[:, :], affT[:])
                o = nc.dram_tensor("o_posm", [NE, T], F32, kind="ExternalOutput").ap()
                P.dma("sp", o[:, :], posm[:])
            P.barrier()
            pk.close()
            with ExitStack() as ph2:
                Per = Ring(ph2, ncw, "Pe", 1, [128, T // 128, NS], BF16)
                xgr = Ring(ph2, ncw, "xg", 1, [128, DC, NS], BF16)
                hdr = Ring(ph2, ncw, "hid", 1, [128, 8, NS], BF16)
                wq = Ring(ph2, ncw, "wq", 4, [128, DC, 512], BF16)
                sg = Ring(ph2, ncw, "sg", 2, [128, NS], F32)
                yt_ = Ring(ph2, ncw, "yt", 3, [128, 512], BF16)
                for e_ in range(NE):
                    Pe = Per.next()
                    for tt_ in range(T // 128):
                        a, b = (0, CAPL) if tt_ < 16 else (CAPL, NS)
                        P.ts("dve", Pe[:, tt_, a:b], iotar[:, a:b],
                             posmt[:, tt_, e_:e_ + 1], None, ALU.is_equal)
                    xg = xgr.next()
                    for dt_ in range(DC):
                        ps = pall.next()
                        for tt_ in range(16):
                            P.mm(ps[:, 0:CAPL], h2tok[:, tt_, dt_ * 128:(dt_ + 1) * 128], Pe[:, tt_, 0:CAPL],
                                 tt_ == 0, tt_ == 15)
                        for tt_ in range(16, 18):
                            P.mm(ps[:, CAPL:NS], h2tok[:, tt_, dt_ * 128:(dt_ + 1) * 128], Pe[:, tt_, CAPL:NS],
                                 tt_ == 16, tt_ == 17)
                        P.copy("act" if dt_ % 2 else "dve", xg[:, dt_, :], ps[:, 0:NS])
                    hid = hdr.next()
                    for hf in range(2):
                        wg_ = wq.next()
                        P.dma("pool", wg_[:], w_gate[l, e_, :, hf * 512:(hf + 1) * 512].rearrange("(c p) f -> p c f", p=128), key="w")
                        wu_ = wq.next()
                        P.dma("pool", wu_[:], w_up[l, e_, :, hf * 512:(hf + 1) * 512].rearrange("(c p) f -> p c f", p=128), key="w")
                        for j in range(4):
                            ft = hf * 4 + j
                            pg = pall.next(); pu = pall.next()
                            for c in range(DC):
                                P.mm(pg[:, 0:NS], wg_[:, c, j * 128:(j + 1) * 128], xg[:, c, :], c == 0, c == DC - 1)
                            for c in range(DC):
                                P.mm(pu[:, 0:NS], wu_[:, c, j * 128:(j + 1) * 128], xg[:, c, :], c == 0, c == DC - 1)
                            s_ = sg.next()
                            P.act(s_[:], pg[:, 0:NS], AF.Silu)
                            P.tt("dve", hid[:, ft, :], s_[:], pu[:, 0:NS], ALU.mult)
                    for db in range(4):
                        wd_ = wq.next()
                        P.dma("pool", wd_[:, 0:8, :], w_down[l, e_, :, db * 512:(db + 1) * 512].rearrange("(c p) d -> p c d", p=128), key="w")
                        for ct, (c0, cn) in enumerate(((0, 128), (128, 128), (256, 32))):
                            ps = pall.next()
                            for f_ in range(8):
                                P.mm(ps[0:cn, :], hid[:, f_, c0:c0 + cn], wd_[:, f_, :], f_ == 0, f_ == 7)
                            y_ = yt_.next()
                            P.copy("act" if ct % 2 else "dve", y_[0:cn, :], ps[0:cn, :])
                            for i in range(4):
                                P.dma("sp", YM[db * 4 + i][0:cn, (e_ * 3 + ct) * 128:(e_ * 3 + ct + 1) * 128],
                                      y_[0:cn, i * 128:(i + 1) * 128])
            P.barrier()
            hk.close()
            with ExitStack() as ph2:
                iotac = ph2.enter_context(ncw.sbuf_tensor("iotac", [128, 3], F32))
                sel = ph2.enter_context(ncw.sbuf_tensor("sel", [NE, NE, 128], F32))
                P.dma("sp", iotac[:], cin["iotac"][:, :])
                P.dma("sp", sel[:], cin["sel"].rearrange("e k m -> k e m"))
                PG = ph2.enter_context(ncw.sbuf_tensor("PG", [128, NE, 2, 1536], BF16))
                gwb = Ring(ph2, ncw, "gwb", 2, [128, 512], F32)
                ymr = Ring(ph2, ncw, "ym", 3, [128, NE * 3 * 128], BF16)
                xr = Ring(ph2, ncw, "sx", 5, [128, 512], F32)
                xo = Ring(ph2, ncw, "so", 3, [128, 512], F32)
                for grp in ((TBLK[0:3], TBLK[3:5]) if stop_after >= 9 else ()):
                    info = []
                    off = 0
                    for (t0, tn) in grp:
                        r = 0 if t0 < NL else 1
                        cts = ((0, 0, 128), (1, 1, 128)) if r == 0 else ((0, 2, 32),)
                        info.append((t0, tn, r, cts, off))
                        for e_ in range(NE):
                            p1 = pps.next(); p2 = pps.next()
                            P.mm(p1[:, 0:tn], sel[:, e_, :], posm[:, t0:t0 + tn], True, True)
                            P.mm(p2[:, 0:tn], sel[:, e_, :], gw[:, t0:t0 + tn], True, True)
                            g_ = gwb.next()
                            P.copy("act", g_[:, 0:tn], p2[:, 0:tn])
                            for (slot, ct, kk) in cts:
                                P.stt(PG[0:kk, e_, slot, off:off + tn], p1[0:kk, 0:tn], iotac[0:kk, ct:ct + 1],
                                      g_[0:kk, 0:tn], ALU.is_equal, ALU.mult)
                        off += tn
                    its = [(dt_, k) for dt_ in range(DC) for k in range(len(info))]
                    ystate = {}

                    def s_loads(it):
                        dt_, k = it
                        t0, tn = info[k][0], info[k][1]
                        if k == 0:
                            ym = ymr.next()
                            P.dma("sp", ym[:], YM[dt_][:, :])
                            ystate[dt_] = ym
                        xt = xr.next()
                        P.dma("sp", xt[:, 0:tn], XT[dt_][:, t0:t0 + tn])
                        return xt

                    pend = [s_loads(its[0]), s_loads(its[1])]
                    for ii, (dt_, k) in enumerate(its):
                        (t0, tn, r, cts, off) = info[k]
                        xt = pend.pop(0)
                        if ii + 2 < len(its):
                            pend.append(s_loads(its[ii + 2]))
                        ym = ystate[dt_]
                        ps = pacc.next()
                        n_mm = NE * len(cts)
                        i_mm = 0
                        for e_ in range(NE):
                            for (slot, ct, kk) in cts:
                                P.mm(ps[:, 0:tn], ym[0:kk, (e_ * 3 + ct) * 128:(e_ * 3 + ct + 1) * 128],
                                     PG[0:kk, e_, slot, off:off + tn], i_mm == 0, i_mm == n_mm - 1)
                                i_mm += 1
                        o_ = xo.next()
                        P.stt(o_[:, 0:tn], ps[:, 0:tn], modT[:, l, 5 * 16 + dt_, r:r + 1], xt[:, 0:tn],
                              ALU.mult, ALU.add)
                        P.dma("sp", XT[dt_][:, t0:t0 + tn], o_[:, 0:tn])
        P.barrier()

    if stop_after >= 9:
        with ExitStack() as ph:
            xr = Ring(ph, ncw, "fx", 2, [128, DC, 256], F32)
            sqr = Ring(ph, ncw, "fsq", 2, [128, 256], BF16)
            rsr = Ring(ph, ncw, "frs", 2, [128, 256], F32)
            tmr = Ring(ph, ncw, "ftm", 3, [128, 256], F32)
            otr = Ring(ph, ncw, "fo", 2, [128, 2, D], F32)
            for t0 in range(0, NL, 256):
                xt = xr.next()
                for c in range(DC):
                    P.dma("sp", xt[:, c, :], XT[c][:, t0:t0 + 256])
                ps = pacc.next()
                for c in range(DC):
                    sq = sqr.next()
                    P.act(sq[:], xt[:, c, :], AF.Square)
                    P.mm(ps[:, 0:256], onesb[:], sq[:], c == 0, c == DC - 1)
                rs = rsr.next()
                P.ts("dve", rs[:], ps[:, 0:256], 1.0 / D, EPS, ALU.mult, ALU.add)
                P.act(rs[:], rs[:], AF.Sqrt)
                P.op("dve", lambda e: e.reciprocal(out=rs[:], in_=rs[:]), r=[rs[:]], w=[rs[:]])
                ot = otr.next()
                for c in range(DC):
                    tm = tmr.next()
                    P.stt(tm[:], xt[:, c, :], gfT[:, c:c + 1], rs[:], ALU.mult, ALU.mult)
                    pt = pps.next()
                    for s_ in range(2):
                        P.tr(pt[:, s_ * 128:(s_ + 1) * 128], tm[:, s_ * 128:(s_ + 1) * 128], ident[:])
                    P.copy("act" if c % 2 else "dve", ot[:, :, c * 128:(c + 1) * 128],
                           pt[:, 0:256].rearrange("p (s d) -> p s d", s=2))
                for s_ in range(2):
                    P.dma("sp", y_out[t0 + s_ * 128:t0 + (s_ + 1) * 128, :], ot[:, s_, :])

    P.barrier()
    st.close()
    return nc, cst


def _host_inputs(inputs, nlayers=L, ncores=8):
    f = lambda a: np.ascontiguousarray(np.asarray(a, dtype=np.float32))
    x = f(inputs["x"]); c = f(inputs["c"]); ctx = f(inputs["ctx"]); c_ctx = f(inputs["c_ctx"])
    _, dri, dci = _att_tables()
    rpb = f(inputs["rpb"])
    g = rpb[:, :, dri, dci]
    rpbg = np.ascontiguousarray(g.transpose(0, 1, 3, 2, 4))
    vecs = np.concatenate([
        f(inputs["b_mod"]).reshape(L, 96, 128),
        f(inputs["g_norm1"]).reshape(L, 16, 128),
        f(inputs["g_norm2"]).reshape(L, 16, 128),
        f(inputs["g_hgrn"]).reshape(L, 8, 128),
        f(inputs["pool_scale"]).reshape(L, 8, 128)], axis=1)
    shared = {
        "w_mod": f(inputs["w_mod"]), "vecs": np.ascontiguousarray(vecs),
        "lbp": f(inputs["lb_param"]).reshape(64, 128), "gfin": f(inputs["g_final"]).reshape(16, 128),
        "w_in": f(inputs["w_in"]), "w_pool": f(inputs["w_pool"]), "rpbg": rpbg,
        "w_branch": f(inputs["w_branch"]), "w_out": f(inputs["w_out"]), "w_router": f(inputs["w_router"]),
        "w_gate_e": f(inputs["w_gate_e"]), "w_up_e": f(inputs["w_up_e"]), "w_down_e": f(inputs["w_down_e"]),
    }
    for k in ("w_mod", "w_in", "w_pool", "rpbg", "w_branch", "w_out", "w_router", "w_gate_e", "w_up_e", "w_down_e"):
        shared[k] = shared[k][:nlayers]
    maps = []
    for core in range(ncores):
        b = core % 4
        m = dict(shared)
        m["x"] = x[b]
        m["ctx"] = ctx[b]
        m["cc"] = np.concatenate([c[b].reshape(16, 128), c_ctx.reshape(16, 128)], axis=0)
        maps.append(m)
    return maps


def kernel(**inputs):
    nc, cst = build()
    maps = _host_inputs(inputs)
    for m in maps:
        for k, v in cst.items():
            m["k_" + k] = v
    res = run_bass_kernel_spmd(nc, maps, core_ids=list(range(8)))
    return np.stack([res.results[b]["y"] for b in range(4)], axis=0).astype(np.float32)
```
